# Optimizing a Trainium2 kernel written in Bass

```python
import jax, jax.numpy as jnp
from jax import lax
import numpy as np

D_MODEL = 1024
BATCH = 8
SEQ = 4096
DEPTH = 1

HEAD_DIM = 64
N_Q_HEADS = 8
N_KV_HEADS = 2
Q_PER_KV = N_Q_HEADS // N_KV_HEADS
ATTN_WIDTH = N_Q_HEADS * HEAD_DIM
KV_WIDTH = N_KV_HEADS * HEAD_DIM
IDX_HEADS = 4
IDX_DIM = 64
TOPK_MAX = 256
Q_BLOCK = 128
RNN_WIDTH = D_MODEL
RNN_BLOCKS = 16
RNN_BLOCK_DIM = RNN_WIDTH // RNN_BLOCKS
CONV_WIDTH = 4
LRU_C = 8.0
D_FF = 2816
FFN_CONV_WIDTH = 3
EPS = 1e-6

IN_WIDTHS = (ATTN_WIDTH, KV_WIDTH, KV_WIDTH, IDX_HEADS * IDX_DIM, IDX_DIM, IDX_HEADS,
             RNN_WIDTH, RNN_WIDTH, D_MODEL, D_MODEL)
IN_TOTAL = ATTN_WIDTH + 2 * KV_WIDTH + IDX_HEADS * IDX_DIM + IDX_DIM + IDX_HEADS + 2 * RNN_WIDTH + 2 * D_MODEL

kernel_name = "hybrid_dsa_rglru_convffn"


def rms_norm(x, g):
    xf = x.astype(jnp.float32)
    y = xf * lax.rsqrt(jnp.mean(xf * xf, axis=-1, keepdims=True) + EPS)
    return (y * g.astype(jnp.float32)).astype(x.dtype)


def causal_dwconv(x, w, b):
    K = w.shape[0]
    L = x.shape[1]
    xp = jnp.pad(x, ((0, 0), (K - 1, 0), (0, 0)))
    y = b
    for j in range(K):
        y = y + w[j] * xp[:, j:j + L]
    return y


def dsa_attention(q, k, v, qi, ki, wi, top_k):
    B, L = q.shape[0], q.shape[1]
    nblk = L // Q_BLOCK

    def to_blocks(a):
        return jnp.moveaxis(a.reshape((B, nblk, Q_BLOCK) + a.shape[2:]), 1, 0)

    key_pos = jnp.arange(L)
    gather = jax.vmap(lambda src, idx: src[idx])

    def one_block(args):
        blk, q_b, qi_b, wi_b = args
        q_pos = blk * Q_BLOCK + jnp.arange(Q_BLOCK)
        causal = key_pos[None, :] <= q_pos[:, None]
        rel = jax.nn.relu(jnp.einsum('bqhd,bsd->bqhs', qi_b, ki).astype(jnp.float32))
        score = jnp.einsum('bqh,bqhs->bqs', wi_b.astype(jnp.float32), rel)
        score = jnp.where(causal[None], score, -jnp.inf)
        _, sel = lax.top_k(score, top_k)
        valid = sel <= q_pos[None, :, None]
        k_sel = gather(k, sel)
        v_sel = gather(v, sel)
        qg = q_b.reshape(B, Q_BLOCK, N_KV_HEADS, Q_PER_KV, HEAD_DIM)
        logits = jnp.einsum('bqgrd,bqkgd->bqgrk', qg, k_sel).astype(jnp.float32) * (HEAD_DIM ** -0.5)
        logits = jnp.where(valid[:, :, None, None, :], logits, -jnp.inf)
        p = jax.nn.softmax(logits, axis=-1).astype(v.dtype)
        o = jnp.einsum('bqgrk,bqkgd->bqgrd', p, v_sel)
        return o.reshape(B, Q_BLOCK, ATTN_WIDTH)

    out = lax.map(one_block, (jnp.arange(nblk), to_blocks(q), to_blocks(qi), to_blocks(wi)))
    return jnp.moveaxis(out, 0, 1).reshape(B, L, ATTN_WIDTH)


def rg_lru(xc, wa, ba, wx, bx, lam):
    B, L, W = xc.shape
    xb = xc.reshape(B, L, RNN_BLOCKS, RNN_BLOCK_DIM)
    r = jax.nn.sigmoid(jnp.einsum('blnd,nde->blne', xb, wa).reshape(B, L, W) + ba)
    i = jax.nn.sigmoid(jnp.einsum('blnd,nde->blne', xb, wx).reshape(B, L, W) + bx)
    log_a = LRU_C * r.astype(jnp.float32) * jax.nn.log_sigmoid(lam.astype(jnp.float32))
    a = jnp.exp(log_a)
    b = jnp.sqrt(-jnp.expm1(2.0 * log_a)) * (i * xc).astype(jnp.float32)

    def step(h, ab):
        h = ab[0] * h + ab[1]
        return h, h

    _, hs = lax.scan(step, jnp.zeros((B, W), jnp.float32),
                     (jnp.swapaxes(a, 0, 1), jnp.swapaxes(b, 0, 1)))
    return jnp.swapaxes(hs, 0, 1).astype(xc.dtype)


def hybrid_layer(x, top_k, norm1_g, w_in, q_norm_g, k_norm_g, kidx_norm_g,
                 conv_w, conv_b, rg_wa, rg_ba, rg_wx, rg_bx, rg_lambda,
                 w_o_attn, w_o_rnn, w_out, norm2_g, w_up, ffn_conv_w, ffn_conv_b, w_down):
    B, L, _ = x.shape
    xn = rms_norm(x, norm1_g)
    proj = xn @ w_in
    split_points = np.cumsum(IN_WIDTHS)[:-1].tolist()
    q, k, v, qi, ki, wi, rx, rgate, ga, gb = jnp.split(proj, split_points, axis=-1)

    q = rms_norm(q.reshape(B, L, N_Q_HEADS, HEAD_DIM), q_norm_g)
    k = rms_norm(k.reshape(B, L, N_KV_HEADS, HEAD_DIM), k_norm_g)
    v = v.reshape(B, L, N_KV_HEADS, HEAD_DIM)
    qi = qi.reshape(B, L, IDX_HEADS, IDX_DIM) * (IDX_DIM ** -0.5)
    ki = rms_norm(ki, kidx_norm_g)
    wi = wi * (IDX_HEADS ** -0.5)
    attn = dsa_attention(q, k, v, qi, ki, wi, top_k)

    xc = causal_dwconv(rx, conv_w, conv_b)
    rnn = rg_lru(xc, rg_wa, rg_ba, rg_wx, rg_bx, rg_lambda) * jax.nn.gelu(rgate)

    merged = jax.nn.sigmoid(ga) * (attn @ w_o_attn) + jax.nn.sigmoid(gb) * (rnn @ w_o_rnn)
    x = x + merged @ w_out

    u = causal_dwconv(rms_norm(x, norm2_g) @ w_up, ffn_conv_w, ffn_conv_b)
    gate, val = jnp.split(u, [D_FF], axis=-1)
    return x + (jax.nn.silu(gate) * val) @ w_down


def setup_inputs(seed: int = 0) -> dict:
    key = jax.random.key(seed)
    ks = jax.random.split(key, 24)
    f32 = jnp.float32

    def nrm(k, shape, scale):
        return jax.random.normal(k, shape, f32) * scale

    def gain(k, shape):
        return 1.0 + 0.02 * jax.random.normal(k, shape, f32)

    a0 = jax.random.uniform(ks[13], (DEPTH, RNN_WIDTH), f32, 0.9, 0.999)
    s = a0 ** (1.0 / LRU_C)
    rg_lambda = jnp.log(s) - jnp.log1p(-s)
    return {
        "x": jax.random.normal(ks[0], (BATCH, SEQ, D_MODEL), f32),
        "norm1_g": gain(ks[1], (DEPTH, D_MODEL)),
        "w_in": nrm(ks[2], (DEPTH, D_MODEL, IN_TOTAL), D_MODEL ** -0.5),
        "q_norm_g": gain(ks[3], (DEPTH, HEAD_DIM)),
        "k_norm_g": gain(ks[4], (DEPTH, HEAD_DIM)),
        "kidx_norm_g": gain(ks[5], (DEPTH, IDX_DIM)),
        "conv_w": nrm(ks[6], (DEPTH, CONV_WIDTH, RNN_WIDTH), CONV_WIDTH ** -0.5),
        "conv_b": nrm(ks[7], (DEPTH, RNN_WIDTH), 0.01),
        "rg_wa": nrm(ks[8], (DEPTH, RNN_BLOCKS, RNN_BLOCK_DIM, RNN_BLOCK_DIM), RNN_BLOCK_DIM ** -0.5),
        "rg_ba": nrm(ks[9], (DEPTH, RNN_WIDTH), 0.01),
        "rg_wx": nrm(ks[10], (DEPTH, RNN_BLOCKS, RNN_BLOCK_DIM, RNN_BLOCK_DIM), RNN_BLOCK_DIM ** -0.5),
        "rg_bx": nrm(ks[11], (DEPTH, RNN_WIDTH), 0.01),
        "rg_lambda": rg_lambda,
        "w_o_attn": nrm(ks[14], (DEPTH, ATTN_WIDTH, D_MODEL), ATTN_WIDTH ** -0.5),
        "w_o_rnn": nrm(ks[15], (DEPTH, RNN_WIDTH, D_MODEL), RNN_WIDTH ** -0.5),
        "w_out": nrm(ks[16], (DEPTH, D_MODEL, D_MODEL), D_MODEL ** -0.5),
        "norm2_g": gain(ks[17], (DEPTH, D_MODEL)),
        "w_up": nrm(ks[18], (DEPTH, D_MODEL, 2 * D_FF), D_MODEL ** -0.5),
        "ffn_conv_w": nrm(ks[19], (DEPTH, FFN_CONV_WIDTH, 2 * D_FF), FFN_CONV_WIDTH ** -0.5),
        "ffn_conv_b": nrm(ks[20], (DEPTH, 2 * D_FF), 0.01),
        "w_down": nrm(ks[21], (DEPTH, D_FF, D_MODEL), D_FF ** -0.5),
    }


def reference(x, norm1_g, w_in, q_norm_g, k_norm_g, kidx_norm_g, conv_w, conv_b,
              rg_wa, rg_ba, rg_wx, rg_bx, rg_lambda, w_o_attn, w_o_rnn, w_out,
              norm2_g, w_up, ffn_conv_w, ffn_conv_b, w_down):
    L = x.shape[1]
    top_k = min(TOPK_MAX, L // 4)
    h = x
    for l in range(DEPTH):
        h = hybrid_layer(h, top_k, norm1_g[l], w_in[l], q_norm_g[l], k_norm_g[l], kidx_norm_g[l],
                         conv_w[l], conv_b[l], rg_wa[l], rg_ba[l], rg_wx[l], rg_bx[l], rg_lambda[l],
                         w_o_attn[l], w_o_rnn[l], w_out[l], norm2_g[l], w_up[l],
                         ffn_conv_w[l], ffn_conv_b[l], w_down[l])
    return h
```

```python
from contextlib import ExitStack

import numpy as np
import concourse.bass as bass
import concourse.mybir as mybir
from concourse.bass_utils import run_bass_kernel_spmd

F32 = mybir.dt.float32
BF16 = mybir.dt.bfloat16
AF = mybir.ActivationFunctionType
ALU = mybir.AluOpType
AX = mybir.AxisListType

L = 4096
D = 1024
NCORES = 8
DFF = 2816
NFF = DFF // 128
IN_TOTAL = 5188
EPS = 1e-6
TOPK = 256
NEG = -30000.0

CV = {}
_o = 0
for _n, _w in (("g1", 8), ("g2", 8), ("convw", 32), ("convb", 8), ("ba", 8), ("bx", 8),
               ("lam", 8), ("fcw", 132), ("fcb", 44), ("qg", 1), ("kg", 1), ("kig", 1)):
    CV[_n] = _o
    _o += _w
NV = _o


class Ev:
    __slots__ = ("sem", "val", "eng")

    def __init__(self, sem, val, eng):
        self.sem, self.val, self.eng = sem, val, eng


class Tile:
    __slots__ = ("name", "w", "r", "dsem", "dcnt")

    def __init__(self, name):
        self.name = name
        self.w = None
        self.r = []
        self.dsem = None
        self.dcnt = 0


class Pool:
    def __init__(self, items):
        self.items = items
        self.i = 0

    def get(self):
        it = self.items[self.i % len(self.items)]
        self.i += 1
        return it


class Prog:
    def __init__(self, nc, es):
        self.nc = nc
        self.es = es
        self.eng = {"pe": nc.tensor, "act": nc.scalar, "dve": nc.vector, "pool": nc.gpsimd, "sp": nc.sync}
        self.sem = {k: es.enter_context(nc.semaphore("sem_" + k)) for k in self.eng}
        self.cnt = {k: 0 for k in self.eng}
        self.waited = {k: {} for k in self.eng}
        self.pending = {k: [] for k in self.eng}
        self.nsem = 0
        self.nwait = 0
        self.dsems = []

    def _wait(self, ek, deps):
        best = {}
        for ev in deps:
            if ev is None:
                continue
            if ev.eng == "pe" and ek == "pe":
                continue
            if ev.val is None:
                raise RuntimeError("dependency on unresolved (non-inc) instruction")
            key = id(ev.sem)
            if key not in best or best[key].val < ev.val:
                best[key] = ev
        w = self.waited[ek]
        for key, ev in best.items():
            if w.get(key, 0) < ev.val:
                self.eng[ek].wait_ge(ev.sem, ev.val)
                w[key] = ev.val
                self.nwait += 1

    def _record(self, ev, reads, writes):
        for t in writes:
            t.w = ev
            t.r = []
        for t in reads:
            t.r = [e for e in t.r if e.sem is not ev.sem] + [ev]

    def op(self, ek, fn, reads=(), writes=(), inc=True):
        deps = []
        for t in reads:
            deps.append(t.w)
        for t in writes:
            deps.append(t.w)
            deps.extend(t.r)
        self._wait(ek, deps)
        ins = fn(self.eng[ek])
        if inc:
            self.cnt[ek] += 1
            ins.then_inc(self.sem[ek], 1)
            ev = Ev(self.sem[ek], self.cnt[ek], ek)
            for p in self.pending[ek]:
                p.val = self.cnt[ek]
            self.pending[ek] = []
        else:
            ev = Ev(self.sem[ek], None, ek)
            self.pending[ek].append(ev)
        self._record(ev, reads, writes)
        return ev

    def dma(self, qk, out, in_, reads=(), writes=(), st=None, **kw):
        deps = []
        for t in reads:
            deps.append(t.w)
        for t in writes:
            deps.append(t.w)
            deps.extend(t.r)
        self._wait(qk, deps)
        if st.dsem is None:
            st.dsem = {}
        if qk not in st.dsem:
            st.dsem[qk] = [self.es.enter_context(self.nc.semaphore("dsem%d" % self.nsem)), 0]
            self.dsems.append(st.dsem[qk])
            self.nsem += 1
        ds = st.dsem[qk]
        ds[1] += 16
        self.eng[qk].dma_start(out=out, in_=in_, **kw).then_inc(ds[0], 16)
        ev = Ev(ds[0], ds[1], "dma")
        self._record(ev, reads, writes)
        return ev

    def barrier(self):
        for ek in self.eng:
            assert not self.pending[ek]
        for ek in self.eng:
            w = self.waited[ek]
            for fk in self.eng:
                if fk != ek and self.cnt[fk] > 0 and w.get(id(self.sem[fk]), 0) < self.cnt[fk]:
                    self.eng[ek].wait_ge(self.sem[fk], self.cnt[fk])
                    w[id(self.sem[fk])] = self.cnt[fk]
            for ds in self.dsems:
                if ds[1] > 0 and w.get(id(ds[0]), 0) < ds[1]:
                    self.eng[ek].wait_ge(ds[0], ds[1])
                    w[id(ds[0])] = ds[1]

    def wait_all(self, ek, tiles):
        deps = []
        for t in tiles:
            deps.append(t.w)
            deps.extend(t.r)
        self._wait(ek, deps)


DBG = {}


def build_nc(debug_x1=False, phases=(1, 2)):
    nc = bass.Bass("TRN2", target_bir_lowering=False)
    x_d = nc.dram_tensor("x", [L, D], F32, kind="ExternalInput").ap()
    cv_d = nc.dram_tensor("cvec", [128, NV], F32, kind="ExternalInput").ap()
    cst_d = nc.dram_tensor("consts", [128, 256], F32, kind="ExternalInput").ap()
    w_in_d = nc.dram_tensor("w_in", [D, IN_TOTAL], F32, kind="ExternalInput").ap()
    wa_d = nc.dram_tensor("rg_wa", [16, 64, 64], F32, kind="ExternalInput").ap()
    wx_d = nc.dram_tensor("rg_wx", [16, 64, 64], F32, kind="ExternalInput").ap()
    woa_d = nc.dram_tensor("w_o_attn", [512, D], F32, kind="ExternalInput").ap()
    wor_d = nc.dram_tensor("w_o_rnn", [D, D], F32, kind="ExternalInput").ap()
    wout_d = nc.dram_tensor("w_out", [D, D], F32, kind="ExternalInput").ap()
    wup_d = nc.dram_tensor("w_up", [D, 2 * DFF], F32, kind="ExternalInput").ap()
    wdn_d = nc.dram_tensor("w_down", [DFF, D], F32, kind="ExternalInput").ap()
    out_d = nc.dram_tensor("out", [L, D], F32, kind="ExternalOutput").ap()
    x1_d = None
    if 1 in phases:
        x1_d = nc.dram_tensor("x1s", [L, D], F32, kind="ExternalOutput" if debug_x1 else "Internal").ap()
        w_in_b = nc.dram_tensor("w_in_b", [D, IN_TOTAL], BF16, kind="Internal").ap()
        woa_b = nc.dram_tensor("woa_b", [512, D], BF16, kind="Internal").ap()
        wor_b = nc.dram_tensor("wor_b", [D, D], BF16, kind="Internal").ap()
        wout_b = nc.dram_tensor("wout_b", [D, D], BF16, kind="Internal").ap()

    with ExitStack() as es:
        P = Prog(nc, es)

        def sb(name, shape, dt):
            return es.enter_context(nc.sbuf_tensor(name, shape, dt))

        def ps(name, shape, dt):
            return es.enter_context(nc.psum_tensor(name, shape, dt))

        cv = sb("cv", [128, NV], F32)
        cv_t = Tile("cv")
        P.dma("sp", cv[:], cv_d, writes=[cv_t], st=cv_t)
        cst = sb("cst", [128, 256], F32)
        cst_t = Tile("cst")
        P.dma("sp", cst[:], cst_d, writes=[cst_t], st=cst_t)
        identb = sb("identb", [128, 128], BF16)
        ident_t = Tile("ident")
        P.op("dve", lambda e: e.tensor_copy(out=identb[:], in_=cst[:, 0:128]), reads=[cst_t], writes=[ident_t])

        pbanks = [ps("pb%d" % i, [128, 512], F32) for i in range(6)]
        pb_tiles = [Tile("pb%d" % i) for i in range(6)]
        ptr = [ps("ptr%d" % i, [128, 1024], BF16) for i in range(2)]
        ptr_t = [Tile("ptr0"), Tile("ptr1")]

        x1_t = [Tile("x1_%d" % i) for i in range(L // 512)]
        if 1 in phases:
            phase1(nc, P, x_d, x1_d, x1_t, w_in_d, wa_d, wx_d, woa_d, wor_d, wout_d, (w_in_b, woa_b, wor_b, wout_b),
                   cv, cv_t, cst, cst_t, identb, ident_t, pbanks, pb_tiles, ptr, ptr_t)
        if 1 in phases and 2 in phases:
            P.barrier()
        if 2 in phases:
            phase2(nc, P, es, sb, x1_t if 1 in phases else None, x1_d if 1 in phases else x_d, out_d, wup_d, wdn_d, cv, cv_t, identb, ident_t,
                   pbanks, pb_tiles, ptr, ptr_t)
    return nc


def phase1(nc, P, x_d, x1_d, x1_t, w_in_d, wa_d, wx_d, woa_d, wor_d, wout_d, scr, cv, cv_t, cst, cst_t,
           identb, ident_t, pbanks, pb_tiles, ptr, ptr_t):
    T = 512
    NS = DBG.get("ns1", L // T)
    NIT = DBG.get("nit", 13)
    w_in_b, woa_b, wor_b, wout_b = scr
    with ExitStack() as es:
        def sb(name, shape, dt):
            return es.enter_context(nc.sbuf_tensor(name, shape, dt))

        NSLOT = 4
        slots = [sb("wslot%d" % i, [128, 8, 512], BF16) for i in range(NSLOT)]
        slot_pool = Pool([(slots[i], Tile("wslot%d" % i)) for i in range(NSLOT)])
        win_v = w_in_d.rearrange("(c p) n -> p c n", p=128)
        winb_v = w_in_b.rearrange("(c p) n -> p c n", p=128)
        woa_v = woa_d.rearrange("(h p) n -> p h n", p=64)
        woab_v = woa_b.rearrange("(h p) n -> p h n", p=64)
        wor_v = wor_d.rearrange("(c p) n -> p c n", p=128)
        worb_v = wor_b.rearrange("(c p) n -> p c n", p=128)
        wout_v = wout_d.rearrange("(c p) n -> p c n", p=128)
        woutb_v = wout_b.rearrange("(c p) n -> p c n", p=128)
        scr_t = {}

        def convert(name, src_v, dst_v, npart, ncols):
            c0 = 0
            while c0 < ncols:
                w = min(512, ncols - c0)
                sl, sl_t = slot_pool.get()
                P.dma("pool", sl[0:npart, :, 0:w], src_v[:, :, c0:c0 + w], writes=[sl_t], st=sl_t)
                t = Tile("scr_%s_%d" % (name, c0))
                scr_t[(name, c0)] = t
                P.dma("sp", dst_v[:, :, c0:c0 + w], sl[0:npart, :, 0:w], reads=[sl_t], writes=[t], st=sl_t)
                c0 += w

        convert("win", win_v, winb_v, 128, IN_TOTAL)
        convert("woa", woa_v, woab_v, 64, D)
        convert("wor", wor_v, worb_v, 128, D)
        convert("wout", wout_v, woutb_v, 128, D)

        def scr_tiles(name, c0, w):
            out = []
            for (n, cc), t in scr_t.items():
                if n == name and cc < c0 + w and c0 < cc + 512:
                    out.append(t)
            return out

        def wload(sl, sl_t, name, view, npart, c0, w, off=0):
            P.dma("sp", sl[0:npart, :, off:off + w], view[:, :, c0:c0 + w], reads=scr_tiles(name, c0, w),
                  writes=[sl_t], st=sl_t)

        KT = sb("KT", [128, 2, L], BF16)
        kt_t = [Tile("kt%d" % i) for i in range(L // T)]
        VA = sb("VA", [128, 32, 2, 128], BF16)
        va_t = [Tile("va%d" % i) for i in range(32)]
        kiT = sb("kiT", [128, L], BF16)
        kit_t = [Tile("kit%d" % i) for i in range(L // T)]
        rxhist = sb("rxhist", [128, 8, 3], F32)
        rxh_t = [Tile("rxh%d" % i) for i in range(8)]
        hst = sb("hst", [128, 8], F32)
        hst_t = [Tile("hst%d" % i) for i in range(8)]
        BDa = sb("BDa", [128, 8, 128], BF16)
        BDx = sb("BDx", [128, 8, 128], BF16)
        bd_t = Tile("bd")
        clam = sb("clam", [128, 16], F32)
        clam_t = Tile("clam")
        I4 = sb("I4", [128, 4, 128], BF16)
        i4_t = Tile("I4")
        ones64 = sb("ones64", [64, 64], BF16)
        ones_t = Tile("ones64")

        P.op("pool", lambda e: e.memset(VA[:, :, :, 64:128], 1.0), writes=va_t)
        P.op("pool", lambda e: e.memset(KT[64:128, :, :], 0.0), writes=kt_t)
        P.op("pool", lambda e: e.memset(kiT[64:128, :], 0.0), writes=kit_t)
        P.op("pool", lambda e: e.memset(rxhist[:], 0.0), writes=rxh_t)
        P.op("pool", lambda e: e.memset(hst[:], 0.0), writes=hst_t)
        P.op("pool", lambda e: e.memset(BDa[:], 0.0), writes=[bd_t])
        P.op("pool", lambda e: e.memset(BDx[:], 0.0), writes=[bd_t])
        P.op("pool", lambda e: e.memset(ones64[:], 1.0 / 64.0), writes=[ones_t])
        for (bd, src) in ((BDa, wa_d), (BDx, wx_d)):
            v = src.rearrange("(m two) d e -> two d m e", two=2)
            P.dma("pool", bd[0:64, :, 0:64], v[0], writes=[bd_t], st=bd_t)
            P.dma("pool", bd[64:128, :, 64:128], v[1], writes=[bd_t], st=bd_t)
        for r in range(4):
            P.op("dve", lambda e, r=r: e.tensor_copy(out=I4[:, r, :], in_=cst[:, 0:128]), reads=[cst_t], writes=[i4_t])
        lam = CV["lam"]
        P.op("act", lambda e: e.activation(out=clam[:, 0:8], in_=cv[:, lam:lam + 8], func=AF.Exp, scale=-1.0),
             reads=[cv_t], writes=[clam_t])
        P.op("act", lambda e: e.activation(out=clam[:, 0:8], in_=clam[:, 0:8], func=AF.Ln, bias=1.0),
             reads=[clam_t], writes=[clam_t])
        P.op("dve", lambda e: e.tensor_scalar(out=clam[:, 8:16], in0=clam[:, 0:8], scalar1=-16.0, scalar2=None, op0=ALU.mult),
             reads=[clam_t], writes=[clam_t])
        P.op("dve", lambda e: e.tensor_scalar(out=clam[:, 0:8], in0=clam[:, 0:8], scalar1=-8.0, scalar2=None, op0=ALU.mult),
             reads=[clam_t], writes=[clam_t])

        big = sb("big", [128, 4096], F32)
        big_t = Tile("big")
        xb = sb("xb", [128, 4, D], F32)
        xb_t = Tile("xb")
        xs = sb("xs1", [128, 4, D], BF16)
        xs_t = Tile("xs1")
        mrgT = xs[:].rearrange("p a (c t) -> p (a c) t", t=T)
        mrg_t = xs_t
        stat = sb("stat1", [128, 8], F32)
        stat_t = Tile("stat1")
        xnTs = [sb("xnT%d" % i, [128, 8, T], BF16) for i in range(2)]
        xnT_ts = [Tile("xnT%d" % i) for i in range(2)]
        qT = sb("qT", [128, 8, T], BF16)
        qT_t = Tile("qT")
        qiT = sb("qiT", [128, 4, T], BF16)
        qiT_t = Tile("qiT")
        P.op("pool", lambda e: e.memset(qT[64:128, :, :], 0.0), writes=[qT_t])
        P.op("pool", lambda e: e.memset(qiT[64:128, :, :], 0.0), writes=[qiT_t])
        wabs = sb("wabs", [128, 4, 4], F32)
        wsgn = sb("wsgn", [128, 4, 4], F32)
        wi_t = [Tile("wi%d" % a) for a in range(4)]
        attnT = sb("attnT", [64, 8, T], BF16)
        attn_t = Tile("attnT")
        rnnT = sb("rnnT", [128, 8, T], BF16)
        rnn_t = Tile("rnnT")
        MBs = [sb("MB%d" % i, [128, 4096], BF16) for i in range(2)]
        mb_ts = [(Tile("MBd%d" % i), Tile("MBa%d" % i)) for i in range(2)]
        bis = sb("bis", [128, 8], F32)
        bis_t = Tile("bis")
        cand = sb("cand", [128, 2], F32)
        cand_t = Tile("cand")
        MS = sb("MS", [128, 16], F32)
        ms_t = Tile("MS")
        stpc = sb("stpc", [128, 16], F32)
        stpc_t = Tile("stpc")
        for k in range(1, 17):
            P.op("pool", lambda e, k=k: e.memset(stpc[:, k - 1:k], 2.0 ** (1 - k)), writes=[stpc_t])
        NF = 8
        ftmp = [sb("ft%d" % i, [128, 3 + T], F32) for i in range(NF)]
        f_pool = Pool([(ftmp[i], Tile("ft%d" % i)) for i in range(NF)])
        NB = 5
        btmp = [sb("bt%d" % i, [128, T], BF16) for i in range(NB)]
        b_pool = Pool([(btmp[i], Tile("bt%d" % i)) for i in range(NB)])
        pb_pool = Pool(list(zip(pbanks[0:4], pb_tiles[0:4])))
        OT = [(pbanks[4], pb_tiles[4]), (pbanks[5], pb_tiles[5])]
        ptr_pool = Pool([(ptr[0], ptr_t[0]), (ptr[1], ptr_t[1])])
        g1 = CV["g1"]
        cw = CV["convw"]

        def proj(pb, pb_t, M, sl, sl_t, c0, xnT, xnT_t):
            for c in range(8):
                P.op("pe", lambda e, c=c: e.matmul(pb[0:M, 0:T], lhsT=sl[:, c, c0:c0 + M], rhs=xnT[:, c, :],
                                                 start=(c == 0), stop=(c == 7)),
                     reads=[xnT_t, sl_t], writes=[pb_t], inc=(c == 7))

        def qknorm(pb, pb_t, out_ap, out_tiles, gcol):
            qf, qf_t = f_pool.get()
            sq, sq_t = b_pool.get()
            P.op("act", lambda e: e.activation(out=qf[0:64, 0:T], in_=pb[0:64, 0:T], func=AF.Copy, scale=cv[0:64, gcol:gcol + 1]),
                 reads=[pb_t, cv_t], writes=[qf_t])
            P.op("act", lambda e: e.activation(out=sq[0:64, :], in_=pb[0:64, 0:T], func=AF.Square), reads=[pb_t], writes=[sq_t])
            p2, p2_t = pb_pool.get()
            P.op("pe", lambda e: e.matmul(p2[0:64, 0:T], lhsT=ones64[:], rhs=sq[0:64, :], start=True, stop=True),
                 reads=[sq_t, ones_t], writes=[p2_t])
            sd, sd_t = f_pool.get()
            P.op("act", lambda e: e.activation(out=sd[0:64, 0:T], in_=p2[0:64, 0:T], func=AF.Ln, bias=EPS), reads=[p2_t], writes=[sd_t])
            P.op("act", lambda e: e.activation(out=sd[0:64, 0:T], in_=sd[0:64, 0:T], func=AF.Exp, scale=-0.5), reads=[sd_t], writes=[sd_t])
            P.op("pool", lambda e: e.tensor_tensor(out=out_ap, in0=qf[0:64, 0:T], in1=sd[0:64, 0:T], op=ALU.mult),
                 reads=[qf_t, sd_t], writes=out_tiles)

        def x_view(s):
            return x_d[s * T:(s + 1) * T, :].rearrange("(a p) d -> p a d", p=128)

        def stage_A(s):
            xnT, xnT_t = xnTs[s % 2], xnT_ts[s % 2]
            P.dma("sp", xb[:], x_view(s), writes=[xb_t], st=xb_t)
            for a in range(4):
                P.op("act", lambda e, a=a: e.activation(out=xs[:, a, :], in_=xb[:, a, :], func=AF.Square,
                                                        accum_out=stat[:, a:a + 1]),
                     reads=[xb_t], writes=[xs_t, stat_t])
            P.op("act", lambda e: e.activation(out=stat[:, 0:4], in_=stat[:, 0:4], func=AF.Ln, scale=1.0 / D, bias=EPS),
                 reads=[stat_t], writes=[stat_t])
            P.op("act", lambda e: e.activation(out=stat[:, 4:8], in_=stat[:, 0:4], func=AF.Exp, scale=-0.5),
                 reads=[stat_t], writes=[stat_t])
            for a in range(4):
                P.op("act", lambda e, a=a: e.activation(out=xs[:, a, :], in_=xb[:, a, :], func=AF.Copy,
                                                        scale=stat[:, 4 + a:5 + a]),
                     reads=[xb_t, stat_t], writes=[xs_t])
            for c in range(8):
                tp, tp_t = ptr_pool.get()
                for a in range(4):
                    P.op("pe", lambda e, a=a, c=c, tp=tp: e.transpose(out=tp[:, a * 128:(a + 1) * 128],
                                                                      in_=xs[:, a, c * 128:(c + 1) * 128],
                                                                      identity=identb[:]),
                         reads=[xs_t, ident_t], writes=[tp_t], inc=(a == 3))
                P.op("act", lambda e, c=c, tp=tp: e.activation(out=xnT[:, c, :], in_=tp[:, 0:T], func=AF.Copy,
                                                               scale=cv[:, g1 + c:g1 + c + 1]),
                     reads=[tp_t, cv_t], writes=[xnT_t])

        def stage_B_idx(s, slk, slk_t, slw, slw_t):
            xnT, xnT_t = xnTs[s % 2], xnT_ts[s % 2]
            tok0 = s * T
            for g in range(2):
                pb, pb_t = pb_pool.get()
                proj(pb, pb_t, 64, slk, slk_t, g * 64, xnT, xnT_t)
                qknorm(pb, pb_t, KT[0:64, g, tok0:tok0 + T], [kt_t[s]], CV["kg"])
            for h in range(4):
                pb, pb_t = pb_pool.get()
                proj(pb, pb_t, 64, slk, slk_t, 256 + h * 64, xnT, xnT_t)
                P.op("act", lambda e, h=h, pb=pb: e.activation(out=qiT[0:64, h, :], in_=pb[0:64, 0:T], func=AF.Copy),
                     reads=[pb_t], writes=[qiT_t])
            pb, pb_t = pb_pool.get()
            proj(pb, pb_t, 64, slw, slw_t, 0, xnT, xnT_t)
            qknorm(pb, pb_t, kiT[0:64, tok0:tok0 + T], [kit_t[s]], CV["kig"])
            for a in range(4):
                j = 4 * s + a
                pb, pb_t = pb_pool.get()
                for c in range(8):
                    P.op("pe", lambda e, c=c, a=a, pb=pb: e.matmul(pb[:, 0:128], lhsT=xnT[:, c, a * 128:(a + 1) * 128],
                                                                 rhs=slk[:, c, 128:256], start=(c == 0), stop=(c == 7)),
                         reads=[xnT_t, slk_t], writes=[pb_t], inc=(c == 7))
                P.op("act", lambda e, j=j, pb=pb: e.activation(out=VA[:, j, :, 0:64],
                                                               in_=pb[:, 0:128].rearrange("p (g d) -> p g d", g=2),
                                                               func=AF.Copy),
                     reads=[pb_t], writes=[va_t[j]])
                pb, pb_t = pb_pool.get()
                for c in range(8):
                    P.op("pe", lambda e, c=c, a=a, pb=pb: e.matmul(pb[:, 0:4], lhsT=xnT[:, c, a * 128:(a + 1) * 128],
                                                                 rhs=slw[:, c, 64:68], start=(c == 0), stop=(c == 7)),
                         reads=[xnT_t, slw_t], writes=[pb_t], inc=(c == 7))
                P.op("act", lambda e, a=a, pb=pb: e.activation(out=wabs[:, a, :], in_=pb[:, 0:4], func=AF.Abs, scale=1.0 / 16.0),
                     reads=[pb_t], writes=[wi_t[a]])
                P.op("act", lambda e, a=a, pb=pb: e.activation(out=wsgn[:, a, :], in_=pb[:, 0:4], func=AF.Sign),
                     reads=[pb_t], writes=[wi_t[a]])

        def stage_B_q(s, slq, slq_t):
            xnT, xnT_t = xnTs[s % 2], xnT_ts[s % 2]
            for h in range(8):
                pb, pb_t = pb_pool.get()
                proj(pb, pb_t, 64, slq, slq_t, h * 64, xnT, xnT_t)
                qknorm(pb, pb_t, qT[0:64, h, :], [qT_t], CV["qg"])

        def att_scores(s, a):
            i = 4 * s + a
            n = 128 * (i + 1)
            qs = slice(a * 128, (a + 1) * 128)
            nch = (n + 511) // 512
            for c in range(nch):
                w = min(512, n - 512 * c)
                ks = slice(512 * c, 512 * c + w)
                for h in range(4):
                    pb, pb_t = pb_pool.get()
                    P.op("pe", lambda e, h=h, pb=pb, w=w, ks=ks: e.matmul(pb[:, 0:w], lhsT=qiT[:, h, qs], rhs=kiT[:, ks],
                                                                         start=True, stop=True),
                         reads=[qiT_t] + kit_t[0:s + 1], writes=[pb_t])
                    r, r_t = f_pool.get()
                    P.op("act", lambda e, h=h, pb=pb, r=r, w=w: e.activation(out=r[:, 0:w], in_=pb[:, 0:w], func=AF.Relu,
                                                                           scale=wabs[:, a, h:h + 1]),
                         reads=[pb_t, wi_t[a]], writes=[r_t])
                    if h == 0:
                        P.op("dve", lambda e, r=r, w=w, ks=ks: e.tensor_scalar(out=big[:, ks], in0=r[:, 0:w],
                                                                              scalar1=wsgn[:, a, 0:1], scalar2=None,
                                                                              op0=ALU.mult),
                             reads=[r_t, wi_t[a]], writes=[big_t])
                    else:
                        P.op("dve", lambda e, h=h, r=r, w=w, ks=ks: e.scalar_tensor_tensor(
                            out=big[:, ks], in0=r[:, 0:w], scalar=wsgn[:, a, h:h + 1], in1=big[:, ks],
                            op0=ALU.mult, op1=ALU.add),
                            reads=[r_t, wi_t[a]], writes=[big_t])
            if i >= 2:
                P.op("dve", lambda e: e.tensor_reduce(out=bis[:, 0:1], in_=big[:, 0:n], axis=AX.X, op=ALU.max,
                                                      apply_absolute_value=True),
                     reads=[big_t], writes=[bis_t])
                P.op("dve", lambda e: e.tensor_scalar(out=bis[:, 1:2], in0=bis[:, 0:1], scalar1=-1.0, scalar2=None, op0=ALU.mult),
                     reads=[bis_t], writes=[bis_t])
            else:
                P.op("dve", lambda e: e.memset(bis[:, 1:2], -1e29), writes=[bis_t])
            P.op("dve", lambda e: e.tensor_tensor(out=big[:, n - 128:n], in0=big[:, n - 128:n], in1=cst[:, 128:256], op=ALU.add),
                 reads=[cst_t, big_t], writes=[big_t])

        def att_bisect(s, a):
            i = 4 * s + a
            n = 128 * (i + 1)
            MB = MBs[i % 2]
            mbD_t, mbA_t = mb_ts[i % 2]
            if i >= 2:
                P.op("dve", lambda e: e.tensor_scalar(out=MS[:, 0:NIT], in0=stpc[:, 0:NIT], scalar1=bis[:, 0:1], scalar2=None, op0=ALU.mult),
                     reads=[bis_t, stpc_t], writes=[ms_t])
                P.op("dve", lambda e: e.memset(cand[:, 0:1], 0.0), writes=[cand_t])
                for k in range(1, NIT + 1):
                    P.op("dve", lambda e: e.tensor_scalar(out=MB[:, 0:n], in0=big[:, 0:n], scalar1=cand[:, 0:1], scalar2=None,
                                                          op0=ALU.is_ge, op1=ALU.add, accum_out=bis[:, 3:4]),
                         reads=[big_t, cand_t], writes=[mbD_t, bis_t])
                    P.op("dve", lambda e: e.tensor_scalar(out=bis[:, 4:5], in0=bis[:, 3:4], scalar1=float(TOPK) - 0.5, scalar2=-0.5,
                                                          op0=ALU.is_ge, op1=ALU.add),
                         reads=[bis_t], writes=[bis_t])
                    P.op("dve", lambda e, k=k: e.scalar_tensor_tensor(out=cand[:, 0:1], in0=bis[:, 4:5], scalar=MS[:, k - 1:k], in1=cand[:, 0:1],
                                                                      op0=ALU.mult, op1=ALU.add),
                         reads=[bis_t, ms_t, cand_t], writes=[cand_t])
                P.op("dve", lambda e: e.scalar_tensor_tensor(out=bis[:, 1:2], in0=MS[:, NIT - 1:NIT], scalar=-0.5, in1=cand[:, 0:1],
                                                             op0=ALU.mult, op1=ALU.add),
                     reads=[ms_t, cand_t], writes=[bis_t])
            P.op("dve", lambda e: e.tensor_scalar(out=MB[:, 0:n], in0=big[:, 0:n], scalar1=bis[:, 1:2], scalar2=NEG,
                                                  op0=ALU.is_lt, op1=ALU.mult),
                 reads=[big_t, bis_t], writes=[mbD_t, mbA_t])

        def att_main(s, a):
            i = 4 * s + a
            qs = slice(a * 128, (a + 1) * 128)
            MB = MBs[i % 2]
            mb_tt = list(mb_ts[i % 2])
            items = [(j, g) for j in range(i + 1) for g in range(2)]

            def st_exp(j, g):
                kb = slice(128 * j, 128 * j + 128)
                pb, pb_t = pb_pool.get()
                P.op("pe", lambda e: e.matmul(pb[:, :], lhsT=KT[:, g, kb], rhs=qT[:, 4 * g:4 * g + 4, qs],
                                              start=True, stop=False),
                     reads=[qT_t] + kt_t[0:s + 1], writes=[pb_t], inc=False)
                P.op("pe", lambda e: e.matmul(pb[:, :], lhsT=MB[:, kb], rhs=I4[:].rearrange("p r q -> p (r q)"),
                                              start=False, stop=True),
                     reads=mb_tt + [i4_t], writes=[pb_t])
                pt, pt_t = b_pool.get()
                P.op("act", lambda e: e.activation(out=pt[:], in_=pb[:, :], func=AF.Exp, scale=0.125),
                     reads=[pb_t], writes=[pt_t])
                return pt, pt_t

            def pv(j, g, pt, pt_t):
                ot, ot_t = OT[g]
                P.op("pe", lambda e: e.matmul(ot[:, :], lhsT=VA[:, j, g, :], rhs=pt[:],
                                              start=(j == 0), stop=(j == i)),
                     reads=[pt_t, va_t[j]], writes=[ot_t], inc=(j == i))

            LOOK = 2
            pend = []
            for k, (j, g) in enumerate(items):
                pend.append((j, g) + st_exp(j, g))
                if len(pend) > LOOK:
                    pv(*pend.pop(0))
            while pend:
                pv(*pend.pop(0))
            for g in range(2):
                ot, ot_t = OT[g]
                rc, rc_t = f_pool.get()
                P.op("act", lambda e, ot=ot, rc=rc: e.activation(out=rc[0:64, 0:T], in_=ot[64:128, :], func=AF.Ln), reads=[ot_t], writes=[rc_t])
                P.op("act", lambda e, rc=rc: e.activation(out=rc[0:64, 0:T], in_=rc[0:64, 0:T], func=AF.Exp, scale=-1.0), reads=[rc_t], writes=[rc_t])
                P.op("dve", lambda e, g=g, ot=ot, rc=rc: e.tensor_tensor(
                    out=attnT[:, 4 * g:4 * g + 4, qs], in0=ot[0:64, :].rearrange("p (r q) -> p r q", r=4),
                    in1=rc[0:64, 0:T].rearrange("p (r q) -> p r q", r=4), op=ALU.mult),
                    reads=[ot_t, rc_t], writes=[attn_t])

        rnn_state = {}
        xc_ded = [(sb("xcd%d" % i, [128, T], F32), Tile("xcd%d" % i)) for i in range(2)]
        xcb_ded = [(sb("xcbd%d" % i, [128, T], BF16), Tile("xcbd%d" % i)) for i in range(2)]

        def rnn_part1(s, m, slr, slr_t):
            xnT, xnT_t = xnTs[s % 2], xnT_ts[s % 2]
            mm = m % 4
            pb, pb_t = pb_pool.get()
            proj(pb, pb_t, 128, slr, slr_t, mm * 128, xnT, xnT_t)
            rxh, rxh_tt = f_pool.get()
            P.op("pool", lambda e: e.tensor_copy(out=rxh[:, 0:3], in_=rxhist[:, m, :]), reads=[rxh_t[m]], writes=[rxh_tt])
            P.op("act", lambda e: e.activation(out=rxh[:, 3:3 + T], in_=pb[:, :], func=AF.Copy), reads=[pb_t], writes=[rxh_tt])
            P.op("pool", lambda e: e.tensor_copy(out=rxhist[:, m, :], in_=rxh[:, T:T + 3]), reads=[rxh_tt], writes=[rxh_t[m]])
            xc, xc_t = xc_ded[m % 2]
            P.op("dve", lambda e: e.tensor_scalar(
                out=xc[:, 0:T], in0=rxh[:, 3:3 + T], scalar1=cv[:, cw + 3 * 8 + m:cw + 3 * 8 + m + 1],
                scalar2=cv[:, CV["convb"] + m:CV["convb"] + m + 1], op0=ALU.mult, op1=ALU.add),
                reads=[rxh_tt, cv_t], writes=[xc_t])
            for jj in (2, 1, 0):
                P.op("dve", lambda e, jj=jj: e.scalar_tensor_tensor(
                    out=xc[:, 0:T], in0=rxh[:, jj:jj + T], scalar=cv[:, cw + jj * 8 + m:cw + jj * 8 + m + 1],
                    in1=xc[:, 0:T], op0=ALU.mult, op1=ALU.add),
                    reads=[rxh_tt, cv_t], writes=[xc_t])
            xcb, xcb_t = xcb_ded[m % 2]
            P.op("act", lambda e: e.activation(out=xcb[:], in_=xc[:, 0:T], func=AF.Copy), reads=[xc_t], writes=[xcb_t])
            rnn_state[m] = (xc, xc_t, xcb, xcb_t)

        def rnn_part2(s, m, slg, slg_t):
            xnT, xnT_t = xnTs[s % 2], xnT_ts[s % 2]
            mm = m % 4
            xc, xc_t, xcb, xcb_t = rnn_state.pop(m)
            pr, pr_t = pb_pool.get()
            P.op("pe", lambda e: e.matmul(pr[:, :], lhsT=BDa[:, m, :], rhs=xcb[:], start=True, stop=True),
                 reads=[xcb_t, bd_t], writes=[pr_t])
            pi, pi_t = pb_pool.get()
            P.op("pe", lambda e: e.matmul(pi[:, :], lhsT=BDx[:, m, :], rhs=xcb[:], start=True, stop=True),
                 reads=[xcb_t, bd_t], writes=[pi_t])
            rr, rr_t = f_pool.get()
            P.op("act", lambda e: e.activation(out=rr[:, 0:T], in_=pr[:, :], func=AF.Sigmoid,
                                               bias=cv[:, CV["ba"] + m:CV["ba"] + m + 1]),
                 reads=[pr_t, cv_t], writes=[rr_t])
            ii, ii_t = f_pool.get()
            P.op("act", lambda e: e.activation(out=ii[:, 0:T], in_=pi[:, :], func=AF.Sigmoid,
                                               bias=cv[:, CV["bx"] + m:CV["bx"] + m + 1]),
                 reads=[pi_t, cv_t], writes=[ii_t])
            aa, aa_t = f_pool.get()
            P.op("act", lambda e: e.activation(out=aa[:, 0:T], in_=rr[:, 0:T], func=AF.Exp, scale=clam[:, m:m + 1]),
                 reads=[rr_t, clam_t], writes=[aa_t])
            P.op("act", lambda e: e.activation(out=rr[:, 0:T], in_=rr[:, 0:T], func=AF.Exp, scale=clam[:, 8 + m:9 + m]),
                 reads=[rr_t, clam_t], writes=[rr_t])
            P.op("act", lambda e: e.activation(out=rr[:, 0:T], in_=rr[:, 0:T], func=AF.Relu, scale=-1.0, bias=1.0),
                 reads=[rr_t], writes=[rr_t])
            P.op("act", lambda e: e.activation(out=rr[:, 0:T], in_=rr[:, 0:T], func=AF.Sqrt, bias=1e-30),
                 reads=[rr_t], writes=[rr_t])
            P.op("pool", lambda e: e.tensor_tensor(out=ii[:, 0:T], in0=ii[:, 0:T], in1=xc[:, 0:T], op=ALU.mult),
                 reads=[ii_t, xc_t], writes=[ii_t])
            P.op("pool", lambda e: e.tensor_tensor(out=ii[:, 0:T], in0=ii[:, 0:T], in1=rr[:, 0:T], op=ALU.mult),
                 reads=[ii_t, rr_t], writes=[ii_t])
            hh, hh_t = f_pool.get()
            P.op("dve", lambda e: e.tensor_tensor_scan(
                out=hh[:, 0:T], data0=aa[:, 0:T], data1=ii[:, 0:T], initial=hst[:, m:m + 1], op0=ALU.mult, op1=ALU.add),
                reads=[aa_t, ii_t, hst_t[m]], writes=[hh_t])
            P.op("dve", lambda e: e.tensor_copy(out=hst[:, m:m + 1], in_=hh[:, T - 1:T]), reads=[hh_t], writes=[hst_t[m]])
            pg, pg_t = pb_pool.get()
            proj(pg, pg_t, 128, slg, slg_t, mm * 128, xnT, xnT_t)
            gl, gl_t = f_pool.get()
            P.op("act", lambda e: e.activation(out=gl[:, 0:T], in_=pg[:, :], func=AF.Gelu_apprx_tanh), reads=[pg_t], writes=[gl_t])
            P.op("dve", lambda e: e.tensor_tensor(out=rnnT[:, m, :], in0=hh[:, 0:T], in1=gl[:, 0:T], op=ALU.mult),
                 reads=[hh_t, gl_t], writes=[rnn_t])

        def merge_pair(s, fp, sg_, sg_t, so_, so_t):
            xnT, xnT_t = xnTs[s % 2], xnT_ts[s % 2]
            for ff in range(2):
                f = 2 * fp + ff
                pga, pga_t = pb_pool.get()
                proj(pga, pga_t, 128, sg_, sg_t, ff * 128, xnT, xnT_t)
                sa, sa_t = f_pool.get()
                P.op("act", lambda e, pga=pga, sa=sa: e.activation(out=sa[:, 0:T], in_=pga[:, :], func=AF.Sigmoid),
                     reads=[pga_t], writes=[sa_t])
                pgb, pgb_t = pb_pool.get()
                proj(pgb, pgb_t, 128, sg_, sg_t, 256 + ff * 128, xnT, xnT_t)
                sbb, sbb_t = f_pool.get()
                P.op("act", lambda e, pgb=pgb, sbb=sbb: e.activation(out=sbb[:, 0:T], in_=pgb[:, :], func=AF.Sigmoid),
                     reads=[pgb_t], writes=[sbb_t])
                pa, pa_t = pb_pool.get()
                for h in range(8):
                    P.op("pe", lambda e, h=h, ff=ff, pa=pa: e.matmul(pa[:, :], lhsT=so_[0:64, h, ff * 128:(ff + 1) * 128],
                                                                    rhs=attnT[:, h, :], start=(h == 0), stop=(h == 7)),
                         reads=[attn_t, so_t], writes=[pa_t], inc=(h == 7))
                P.op("dve", lambda e, sa=sa, pa=pa: e.tensor_tensor(out=sa[:, 0:T], in0=sa[:, 0:T], in1=pa[:, :], op=ALU.mult),
                     reads=[sa_t, pa_t], writes=[sa_t])
                pbm, pbm_t = pb_pool.get()
                for c in range(8):
                    P.op("pe", lambda e, c=c, ff=ff, pbm=pbm: e.matmul(pbm[:, :], lhsT=so_[:, c, 256 + ff * 128:256 + (ff + 1) * 128],
                                                                      rhs=rnnT[:, c, :], start=(c == 0), stop=(c == 7)),
                         reads=[rnn_t, so_t], writes=[pbm_t], inc=(c == 7))
                P.op("dve", lambda e, sbb=sbb, pbm=pbm: e.tensor_tensor(out=sbb[:, 0:T], in0=sbb[:, 0:T], in1=pbm[:, :], op=ALU.mult),
                     reads=[sbb_t, pbm_t], writes=[sbb_t])
                P.op("dve", lambda e, f=f, sa=sa, sbb=sbb: e.tensor_tensor(out=mrgT[:, f, :], in0=sa[:, 0:T], in1=sbb[:, 0:T], op=ALU.add),
                     reads=[sa_t, sbb_t], writes=[mrg_t])

        def outproj_half(s, nn, swo, swo_t):
            for a in range(4):
                pb, pb_t = pb_pool.get()
                for f in range(8):
                    P.op("pe", lambda e, a=a, f=f, pb=pb: e.matmul(pb[:, :], lhsT=mrgT[:, f, a * 128:(a + 1) * 128],
                                                                  rhs=swo[:, f, :], start=(f == 0), stop=(f == 7)),
                         reads=[mrg_t, swo_t], writes=[pb_t], inc=(f == 7))
                P.op("dve", lambda e, a=a, pb=pb: e.tensor_tensor(out=xb[:, a, nn * 512:(nn + 1) * 512], in0=pb[:, :],
                                                                 in1=xb[:, a, nn * 512:(nn + 1) * 512], op=ALU.add),
                     reads=[pb_t, xb_t], writes=[xb_t])

        slot_free = list(slot_pool.items)

        class _SP:
            @staticmethod
            def get():
                assert slot_free, "no free weight slot"
                return slot_free.pop(0)

        def release(w):
            for k in range(0, len(w), 2):
                slot_free.append((w[k], w[k + 1]))

        def L_rnn(half):
            def f():
                a_ = _SP.get(); wload(a_[0], a_[1], "win", winb_v, 128, 1092 + 512 * half, 512)
                b_ = _SP.get(); wload(b_[0], b_[1], "win", winb_v, 128, 2116 + 512 * half, 512)
                return a_ + b_
            return f

        def L_idx():
            a_ = _SP.get(); wload(a_[0], a_[1], "win", winb_v, 128, 512, 512)
            b_ = _SP.get(); wload(b_[0], b_[1], "win", winb_v, 128, 1024, 68)
            return a_ + b_

        def L_q():
            a_ = _SP.get(); wload(a_[0], a_[1], "win", winb_v, 128, 0, 512)
            return a_

        def L_merge(fp):
            def f():
                a_ = _SP.get()
                wload(a_[0], a_[1], "win", winb_v, 128, 3140 + 256 * fp, 256, off=0)
                wload(a_[0], a_[1], "win", winb_v, 128, 4164 + 256 * fp, 256, off=256)
                b_ = _SP.get()
                wload(b_[0], b_[1], "woa", woab_v, 64, 256 * fp, 256, off=0)
                wload(b_[0], b_[1], "wor", worb_v, 128, 256 * fp, 256, off=256)
                return a_ + b_
            return f

        def L_out(nn):
            def f():
                a_ = _SP.get(); wload(a_[0], a_[1], "wout", woutb_v, 128, 512 * nn, 512)
                return a_
            return f

        stage_A(0)
        w_idx = L_idx()
        w_q = L_q()
        stage_B_idx(0, *w_idx)
        release(w_idx)
        stage_B_q(0, *w_q)
        release(w_q)
        w_r0 = L_rnn(0)()
        att_scores(0, 0)
        att_bisect(0, 0)
        for s in range(NS):
            last = (s == NS - 1)
            w_r1 = None
            for a in range(4):
                if a == 0:
                    w_r1 = L_rnn(1)()
                if a == 2 and not last:
                    w_idx = L_idx()
                if a == 3 and not last:
                    w_q = L_q()
                wr = w_r0 if a < 2 else w_r1
                for m in (2 * a, 2 * a + 1):
                    rnn_part1(s, m, wr[0], wr[1])
                if a < 3:
                    att_scores(s, a + 1)
                    att_bisect(s, a + 1)
                if a == 2 and not last:
                    stage_A(s + 1)
                    stage_B_idx(s + 1, *w_idx)
                    release(w_idx)
                if a == 3 and not last:
                    att_scores(s + 1, 0)
                    att_bisect(s + 1, 0)
                att_main(s, a)
                for m in (2 * a, 2 * a + 1):
                    rnn_part2(s, m, wr[2], wr[3])
                if a == 1:
                    release(w_r0)
                if a == 3:
                    release(w_r1)
            w_m0 = L_merge(0)()
            if not last:
                stage_B_q(s + 1, *w_q)
                release(w_q)
            w_m1 = L_merge(1)()
            merge_pair(s, 0, *w_m0)
            release(w_m0)
            w_m2 = L_merge(2)()
            merge_pair(s, 1, *w_m1)
            release(w_m1)
            w_m3 = L_merge(3)()
            merge_pair(s, 2, *w_m2)
            release(w_m2)
            w_o0 = L_out(0)()
            merge_pair(s, 3, *w_m3)
            release(w_m3)
            w_o1 = L_out(1)()
            P.dma("sp", xb[:], x_view(s), writes=[xb_t], st=xb_t)
            outproj_half(s, 0, *w_o0)
            release(w_o0)
            if not last:
                w_r0 = L_rnn(0)()
            outproj_half(s, 1, *w_o1)
            release(w_o1)
            P.dma("sp", x1_d[s * T:(s + 1) * T, :].rearrange("(a p) d -> p a d", p=128), xb[:],
                  reads=[xb_t], writes=[x1_t[s]], st=xb_t)
        P.wait_all("sp", [xb_t] + x1_t)


def phase2(nc, P, es2, sb_outer, x1_t, x1_d, out_d, wup_d, wdn_d, cv, cv_t, identb, ident_t, pbanks, pb_tiles, ptr, ptr_t):
    T2 = 256
    NS2 = L // T2
    with ExitStack() as es:
        def sb(name, shape, dt):
            return es.enter_context(nc.sbuf_tensor(name, shape, dt))

        wup = sb("wup", [128, 8, 2 * DFF], BF16)
        wdn = sb("wdn", [128, NFF, D], BF16)
        wup_t = [Tile("wup%d" % i) for i in range(11)]
        wdn_t = [Tile("wdn%d" % i) for i in range(2)]
        wup_v = wup_d.rearrange("(c p) n -> p c n", p=128)
        wdn_v = wdn_d.rearrange("(f p) n -> p f n", p=128)
        for i in range(11):
            P.dma("pool", wup[:, :, i * 512:(i + 1) * 512], wup_v[:, :, i * 512:(i + 1) * 512],
                  writes=[wup_t[i]], st=wup_t[i])
        for i in range(2):
            P.dma("pool", wdn[:, :, i * 512:(i + 1) * 512], wdn_v[:, :, i * 512:(i + 1) * 512],
                  writes=[wdn_t[i]], st=wdn_t[i])

        hist = sb("hist", [128, 2 * NFF, 2], F32)
        hist_t = [Tile("hist%d" % i) for i in range(2 * NFF)]
        hist_all = Tile("hist_all")
        P.op("pool", lambda e: e.memset(hist[:], 0.0), writes=hist_t)

        xts = [sb("xt%d" % i, [128, 2, D], F32) for i in range(3)]
        xt_pool = Pool([(xts[i], Tile("xt%d" % i)) for i in range(3)])
        xs = sb("xs", [128, 2, D], BF16)
        xs_t = Tile("xs")
        stat = sb("stat2", [128, 8], F32)
        stat_pool = Pool([(stat[:, 0:2], stat[:, 2:4], Tile("st2a")), (stat[:, 4:6], stat[:, 6:8], Tile("st2b"))])
        xnTs = [sb("xn2T%d" % i, [128, 8, T2], BF16) for i in range(2)]
        xnT_ts = [Tile("xn2T%d" % i) for i in range(2)]
        hTs = [sb("hT%d" % i, [128, NFF, T2], BF16) for i in range(2)]
        hT_ts = [Tile("hT%d" % i) for i in range(2)]
        upbs = [sb("upb%d" % i, [128, 2 + T2], F32) for i in range(6)]
        upb_pool = Pool([(upbs[i], Tile("upb%d" % i)) for i in range(6)])
        us = [sb("u%d" % i, [128, T2], F32) for i in range(6)]
        u_pool = Pool([(us[i], Tile("u%d" % i)) for i in range(6)])
        sgs = [sb("sg%d" % i, [128, T2], F32) for i in range(3)]
        sg_pool = Pool([(sgs[i], Tile("sg%d" % i)) for i in range(3)])
        pb_pool = Pool(list(zip(pbanks[0:5], pb_tiles[0:5])))
        pdn, pdn_t = pbanks[5], pb_tiles[5]
        ptr_pool = Pool([(ptr[0], ptr_t[0]), (ptr[1], ptr_t[1])])
        fcw = CV["fcw"]
        fcb = CV["fcb"]
        g2 = CV["g2"]
        NS2 = DBG.get('ns2', NS2)

        def load_x(s):
            xt, xt_t = xt_pool.get()
            P.dma("sp", xt[:], x1_d[s * T2:(s + 1) * T2, :].rearrange("(a p) d -> p a d", p=128),
                  reads=([x1_t[s // 2]] if x1_t is not None else []), writes=[xt_t], st=xt_t)
            return xt, xt_t

        def stage_A2(s, xt, xt_t):
            xnT, xnT_t = xnTs[s % 2], xnT_ts[s % 2]
            ss, rs, st_t = stat_pool.get()
            for a in range(2):
                P.op("act", lambda e, a=a: e.activation(out=xs[:, a, :], in_=xt[:, a, :], func=AF.Square,
                                                        accum_out=ss[:, a:a + 1]),
                     reads=[xt_t], writes=[xs_t, st_t])
            P.op("act", lambda e: e.activation(out=ss, in_=ss, func=AF.Ln, scale=1.0 / D, bias=EPS),
                 reads=[st_t], writes=[st_t])
            P.op("act", lambda e: e.activation(out=rs, in_=ss, func=AF.Exp, scale=-0.5), reads=[st_t], writes=[st_t])
            for a in range(2):
                P.op("act", lambda e, a=a: e.activation(out=xs[:, a, :], in_=xt[:, a, :], func=AF.Copy,
                                                        scale=rs[:, a:a + 1]),
                     reads=[xt_t, st_t], writes=[xs_t])
            for c in range(8):
                tp, tp_t = ptr_pool.get()
                for a in range(2):
                    P.op("pe", lambda e, a=a, c=c, tp=tp: e.transpose(out=tp[:, a * 128:(a + 1) * 128],
                                                                      in_=xs[:, a, c * 128:(c + 1) * 128],
                                                                      identity=identb[:]),
                         reads=[xs_t, ident_t], writes=[tp_t], inc=(a == 1))
                P.op("act", lambda e, c=c, tp=tp: e.activation(out=xnT[:, c, :], in_=tp[:, 0:T2], func=AF.Copy,
                                                               scale=cv[:, g2 + c:g2 + c + 1]),
                     reads=[tp_t, cv_t], writes=[xnT_t])

        def down_ops(s, xt, xt_t):
            hT, hT_t = hTs[s % 2], hT_ts[s % 2]
            for a in range(2):
                for n in range(2):
                    for f in range(NFF):
                        P.op("pe", lambda e, a=a, n=n, f=f: e.matmul(pdn[:, :], lhsT=hT[:, f, a * 128:(a + 1) * 128],
                                                                   rhs=wdn[:, f, n * 512:(n + 1) * 512],
                                                                   start=(f == 0), stop=(f == NFF - 1)),
                             reads=[hT_t, wdn_t[n]], writes=[pdn_t], inc=(f == NFF - 1))
                        if f == NFF - 1:
                            P.op("dve", lambda e, a=a, n=n: e.tensor_tensor(out=xt[:, a, n * 512:(n + 1) * 512], in0=pdn[:, :],
                                                                           in1=xt[:, a, n * 512:(n + 1) * 512], op=ALU.add),
                                 reads=[pdn_t, xt_t], writes=[xt_t])
                        yield
            P.dma("sp", out_d[s * T2:(s + 1) * T2, :].rearrange("(a p) d -> p a d", p=128), xt[:],
                  reads=[xt_t], st=xt_t)
            yield

        def pull(gen, k):
            if gen is None:
                return None
            for _ in range(k):
                try:
                    next(gen)
                except StopIteration:
                    return None
            return gen

        cur = load_x(0)
        stage_A2(0, *cur)
        dgen = None
        for s in range(NS2):
            xt, xt_t = cur
            xnT, xnT_t = xnTs[s % 2], xnT_ts[s % 2]
            hT, hT_t = hTs[s % 2], hT_ts[s % 2]
            if s + 1 < NS2:
                nxt = load_x(s + 1)
            for f in range(NFF):
                uu = []
                for ff in (f, NFF + f):
                    pb, pb_t = pb_pool.get()
                    for c in range(8):
                        P.op("pe", lambda e, c=c, ff=ff, pb=pb: e.matmul(pb[:, 0:T2], lhsT=wup[:, c, ff * 128:(ff + 1) * 128],
                                                                       rhs=xnT[:, c, :], start=(c == 0), stop=(c == 7)),
                             reads=[xnT_t, wup_t[ff // 4]], writes=[pb_t], inc=(c == 7))
                    upb, upb_t = upb_pool.get()
                    P.op("pool", lambda e, upb=upb, ff=ff: e.tensor_copy(out=upb[:, 0:2], in_=hist[:, ff, :]),
                         reads=[hist_t[ff]], writes=[upb_t])
                    P.op("act", lambda e, upb=upb, pb=pb: e.activation(out=upb[:, 2:2 + T2], in_=pb[:, 0:T2], func=AF.Copy),
                         reads=[pb_t], writes=[upb_t])
                    P.op("pool", lambda e, upb=upb, ff=ff: e.tensor_copy(out=hist[:, ff, :], in_=upb[:, T2:T2 + 2]),
                         reads=[upb_t], writes=[hist_t[ff]])
                    u, u_t = u_pool.get()
                    P.op("act", lambda e, u=u, pb=pb, ff=ff: e.activation(
                        out=u[:], in_=pb[:, 0:T2], func=AF.Identity, scale=cv[:, fcw + 2 * 44 + ff:fcw + 2 * 44 + ff + 1],
                        bias=cv[:, fcb + ff:fcb + ff + 1]),
                        reads=[pb_t, cv_t], writes=[u_t])
                    for j in (1, 0):
                        P.op("dve", lambda e, u=u, upb=upb, ff=ff, j=j: e.scalar_tensor_tensor(
                            out=u[:], in0=upb[:, j:j + T2], scalar=cv[:, fcw + j * 44 + ff:fcw + j * 44 + ff + 1],
                            in1=u[:], op0=ALU.mult, op1=ALU.add),
                            reads=[upb_t, cv_t], writes=[u_t])
                    uu.append((u, u_t))
                sg, sg_t = sg_pool.get()
                P.op("act", lambda e, sg=sg, u=uu[0][0]: e.activation(out=sg[:], in_=u[:], func=AF.Silu),
                     reads=[uu[0][1]], writes=[sg_t])
                P.op("dve", lambda e, sg=sg, u=uu[1][0], f=f: e.tensor_tensor(out=hT[:, f, :], in0=sg[:], in1=u[:], op=ALU.mult),
                     reads=[sg_t, uu[1][1]], writes=[hT_t])
                dgen = pull(dgen, 4 if f < NFF - 1 else 1000)
                if f == 8 and s + 1 < NS2:
                    stage_A2(s + 1, *nxt)
            dgen = down_ops(s, xt, xt_t)
            if s + 1 < NS2:
                cur = nxt
        pull(dgen, 1000)
        P.wait_all("sp", [t for _, t in xt_pool.items])


def pack_cvec(inp):
    cvv = np.zeros((128, NV), np.float32)

    def put(name, vec, width):
        cvv[:, CV[name]:CV[name] + width] = np.asarray(vec, np.float32).reshape(width, 128).T

    put("g1", inp["norm1_g"][0], 8)
    put("g2", inp["norm2_g"][0], 8)
    put("convw", inp["conv_w"][0].reshape(-1), 32)
    put("convb", inp["conv_b"][0], 8)
    put("ba", inp["rg_ba"][0], 8)
    put("bx", inp["rg_bx"][0], 8)
    put("lam", inp["rg_lambda"][0], 8)
    put("fcw", inp["ffn_conv_w"][0].reshape(-1), 132)
    put("fcb", inp["ffn_conv_b"][0], 44)
    cvv[0:64, CV["qg"]] = np.asarray(inp["q_norm_g"][0], np.float32)
    cvv[0:64, CV["kg"]] = np.asarray(inp["k_norm_g"][0], np.float32)
    cvv[0:64, CV["kig"]] = np.asarray(inp["kidx_norm_g"][0], np.float32)
    return cvv


def make_in_maps(inp):
    cvv = pack_cvec(inp)
    cst = np.zeros((128, 256), np.float32)
    cst[:, 0:128] = np.eye(128, dtype=np.float32)
    cst[:, 128:256] = np.where(np.arange(128)[None, :] <= np.arange(128)[:, None], 0.0, -1e30)
    shared = {
        "cvec": cvv,
        "consts": cst,
        "w_in": np.ascontiguousarray(inp["w_in"][0], dtype=np.float32),
        "rg_wa": np.ascontiguousarray(inp["rg_wa"][0], dtype=np.float32),
        "rg_wx": np.ascontiguousarray(inp["rg_wx"][0], dtype=np.float32),
        "w_o_attn": np.ascontiguousarray(inp["w_o_attn"][0], dtype=np.float32),
        "w_o_rnn": np.ascontiguousarray(inp["w_o_rnn"][0], dtype=np.float32),
        "w_out": np.ascontiguousarray(inp["w_out"][0], dtype=np.float32),
        "w_up": np.ascontiguousarray(inp["w_up"][0], dtype=np.float32),
        "w_down": np.ascontiguousarray(inp["w_down"][0], dtype=np.float32),
    }
    x = np.asarray(inp["x"], np.float32)
    maps = []
    for b in range(NCORES):
        m = dict(shared)
        m["x"] = np.ascontiguousarray(x[b])
        maps.append(m)
    return maps


def kernel(**inputs):
    nc = build_nc()
    in_maps = make_in_maps(inputs)
    res = run_bass_kernel_spmd(nc, in_maps, core_ids=list(range(NCORES)))
    return np.stack([np.asarray(r["out"], np.float32) for r in res.results], axis=0)
```

```python
from contextlib import ExitStack

import numpy as np
import concourse.bass as bass
import concourse.mybir as mybir
from concourse.bass_utils import run_bass_kernel_spmd

F32 = mybir.dt.float32
BF16 = mybir.dt.bfloat16
AF = mybir.ActivationFunctionType
ALU = mybir.AluOpType
AX = mybir.AxisListType

L = 4096
D = 1024
NCORES = 8
DFF = 2816
NFF = DFF // 128
IN_TOTAL = 5188
EPS = 1e-6
TOPK = 256
NEG = -30000.0

CV = {}
_o = 0
for _n, _w in (("g1", 8), ("g2", 8), ("convw", 32), ("convb", 8), ("ba", 8), ("bx", 8),
               ("lam", 8), ("fcw", 132), ("fcb", 44), ("qg", 1), ("kg", 1), ("kig", 1)):
    CV[_n] = _o
    _o += _w
NV = _o


class Ev:
    __slots__ = ("sem", "val", "eng")

    def __init__(self, sem, val, eng):
        self.sem, self.val, self.eng = sem, val, eng


class Tile:
    __slots__ = ("name", "w", "r", "dsem", "dcnt")

    def __init__(self, name):
        self.name = name
        self.w = None
        self.r = []
        self.dsem = None
        self.dcnt = 0


class Pool:
    def __init__(self, items):
        self.items = items
        self.i = 0

    def get(self):
        it = self.items[self.i % len(self.items)]
        self.i += 1
        return it


class Prog:
    def __init__(self, nc, es):
        self.nc = nc
        self.es = es
        self.eng = {"pe": nc.tensor, "act": nc.scalar, "dve": nc.vector, "pool": nc.gpsimd, "sp": nc.sync}
        self.sem = {k: es.enter_context(nc.semaphore("sem_" + k)) for k in self.eng}
        self.cnt = {k: 0 for k in self.eng}
        self.waited = {k: {} for k in self.eng}
        self.pending = {k: [] for k in self.eng}
        self.nsem = 0
        self.nwait = 0
        self.dsems = []

    def _wait(self, ek, deps):
        best = {}
        for ev in deps:
            if ev is None:
                continue
            if ev.eng == "pe" and ek == "pe":
                continue
            if ev.val is None:
                raise RuntimeError("dependency on unresolved (non-inc) instruction")
            key = id(ev.sem)
            if key not in best or best[key].val < ev.val:
                best[key] = ev
        w = self.waited[ek]
        for key, ev in best.items():
            if w.get(key, 0) < ev.val:
                self.eng[ek].wait_ge(ev.sem, ev.val)
                w[key] = ev.val
                self.nwait += 1

    def _record(self, ev, reads, writes):
        for t in writes:
            t.w = ev
            t.r = []
        for t in reads:
            t.r = [e for e in t.r if e.sem is not ev.sem] + [ev]

    def op(self, ek, fn, reads=(), writes=(), inc=True):
        deps = []
        for t in reads:
            deps.append(t.w)
        for t in writes:
            deps.append(t.w)
            deps.extend(t.r)
        self._wait(ek, deps)
        ins = fn(self.eng[ek])
        if inc:
            self.cnt[ek] += 1
            ins.then_inc(self.sem[ek], 1)
            ev = Ev(self.sem[ek], self.cnt[ek], ek)
            for p in self.pending[ek]:
                p.val = self.cnt[ek]
            self.pending[ek] = []
        else:
            ev = Ev(self.sem[ek], None, ek)
            self.pending[ek].append(ev)
        self._record(ev, reads, writes)
        return ev

    def dma(self, qk, out, in_, reads=(), writes=(), st=None, **kw):
        deps = []
        for t in reads:
            deps.append(t.w)
        for t in writes:
            deps.append(t.w)
            deps.extend(t.r)
        self._wait(qk, deps)
        if st.dsem is None:
            st.dsem = {}
        if qk not in st.dsem:
            st.dsem[qk] = [self.es.enter_context(self.nc.semaphore("dsem%d" % self.nsem)), 0]
            self.dsems.append(st.dsem[qk])
            self.nsem += 1
        ds = st.dsem[qk]
        ds[1] += 16
        self.eng[qk].dma_start(out=out, in_=in_, **kw).then_inc(ds[0], 16)
        ev = Ev(ds[0], ds[1], "dma")
        self._record(ev, reads, writes)
        return ev

    def barrier(self):
        for ek in self.eng:
            assert not self.pending[ek]
        for ek in self.eng:
            w = self.waited[ek]
            for fk in self.eng:
                if fk != ek and self.cnt[fk] > 0 and w.get(id(self.sem[fk]), 0) < self.cnt[fk]:
                    self.eng[ek].wait_ge(self.sem[fk], self.cnt[fk])
                    w[id(self.sem[fk])] = self.cnt[fk]
            for ds in self.dsems:
                if ds[1] > 0 and w.get(id(ds[0]), 0) < ds[1]:
                    self.eng[ek].wait_ge(ds[0], ds[1])
                    w[id(ds[0])] = ds[1]

    def wait_all(self, ek, tiles):
        deps = []
        for t in tiles:
            deps.append(t.w)
            deps.extend(t.r)
        self._wait(ek, deps)


DBG = {}


def build_nc(debug_x1=False, phases=(1, 2)):
    nc = bass.Bass("TRN2", target_bir_lowering=False)
    x_d = nc.dram_tensor("x", [L, D], F32, kind="ExternalInput").ap()
    cv_d = nc.dram_tensor("cvec", [128, NV], F32, kind="ExternalInput").ap()
    cst_d = nc.dram_tensor("consts", [128, 256], F32, kind="ExternalInput").ap()
    w_in_d = nc.dram_tensor("w_in", [D, IN_TOTAL], F32, kind="ExternalInput").ap()
    wa_d = nc.dram_tensor("rg_wa", [16, 64, 64], F32, kind="ExternalInput").ap()
    wx_d = nc.dram_tensor("rg_wx", [16, 64, 64], F32, kind="ExternalInput").ap()
    woa_d = nc.dram_tensor("w_o_attn", [512, D], F32, kind="ExternalInput").ap()
    wor_d = nc.dram_tensor("w_o_rnn", [D, D], F32, kind="ExternalInput").ap()
    wout_d = nc.dram_tensor("w_out", [D, D], F32, kind="ExternalInput").ap()
    wup_d = nc.dram_tensor("w_up", [D, 2 * DFF], F32, kind="ExternalInput").ap()
    wdn_d = nc.dram_tensor("w_down", [DFF, D], F32, kind="ExternalInput").ap()
    out_d = nc.dram_tensor("out", [L, D], F32, kind="ExternalOutput").ap()
    x1_d = None
    if 1 in phases:
        x1_d = nc.dram_tensor("x1s", [L, D], F32, kind="ExternalOutput" if debug_x1 else "Internal").ap()
        w_in_b = nc.dram_tensor("w_in_b", [D, IN_TOTAL], BF16, kind="Internal").ap()
        woa_b = nc.dram_tensor("woa_b", [512, D], BF16, kind="Internal").ap()
        wor_b = nc.dram_tensor("wor_b", [D, D], BF16, kind="Internal").ap()
        wout_b = nc.dram_tensor("wout_b", [D, D], BF16, kind="Internal").ap()

    with ExitStack() as es:
        P = Prog(nc, es)

        def sb(name, shape, dt):
            return es.enter_context(nc.sbuf_tensor(name, shape, dt))

        def ps(name, shape, dt):
            return es.enter_context(nc.psum_tensor(name, shape, dt))

        cv = sb("cv", [128, NV], F32)
        cv_t = Tile("cv")
        P.dma("sp", cv[:], cv_d, writes=[cv_t], st=cv_t)
        cst = sb("cst", [128, 256], F32)
        cst_t = Tile("cst")
        P.dma("sp", cst[:], cst_d, writes=[cst_t], st=cst_t)
        identb = sb("identb", [128, 128], BF16)
        ident_t = Tile("ident")
        P.op("dve", lambda e: e.tensor_copy(out=identb[:], in_=cst[:, 0:128]), reads=[cst_t], writes=[ident_t])

        pbanks = [ps("pb%d" % i, [128, 512], F32) for i in range(6)]
        pb_tiles = [Tile("pb%d" % i) for i in range(6)]
        ptr = [ps("ptr%d" % i, [128, 1024], BF16) for i in range(2)]
        ptr_t = [Tile("ptr0"), Tile("ptr1")]

        x1_t = [Tile("x1_%d" % i) for i in range(L // 512)]
        if 1 in phases:
            phase1(nc, P, x_d, x1_d, x1_t, w_in_d, wa_d, wx_d, woa_d, wor_d, wout_d, (w_in_b, woa_b, wor_b, wout_b),
                   cv, cv_t, cst, cst_t, identb, ident_t, pbanks, pb_tiles, ptr, ptr_t)
        if 1 in phases and 2 in phases:
            P.barrier()
        if 2 in phases:
            phase2(nc, P, es, sb, x1_t if 1 in phases else None, x1_d if 1 in phases else x_d, out_d, wup_d, wdn_d, cv, cv_t, identb, ident_t,
                   pbanks, pb_tiles, ptr, ptr_t)
    return nc


def phase1(nc, P, x_d, x1_d, x1_t, w_in_d, wa_d, wx_d, woa_d, wor_d, wout_d, scr, cv, cv_t, cst, cst_t,
           identb, ident_t, pbanks, pb_tiles, ptr, ptr_t):
    T = 512
    NS = DBG.get("ns1", L // T)
    NIT = DBG.get("nit", 13)
    w_in_b, woa_b, wor_b, wout_b = scr
    with ExitStack() as es:
        def sb(name, shape, dt):
            return es.enter_context(nc.sbuf_tensor(name, shape, dt))

        NSLOT = 4
        slots = [sb("wslot%d" % i, [128, 8, 512], BF16) for i in range(NSLOT)]
        slot_pool = Pool([(slots[i], Tile("wslot%d" % i)) for i in range(NSLOT)])
        win_v = w_in_d.rearrange("(c p) n -> p c n", p=128)
        winb_v = w_in_b.rearrange("(c p) n -> p c n", p=128)
        woa_v = woa_d.rearrange("(h p) n -> p h n", p=64)
        woab_v = woa_b.rearrange("(h p) n -> p h n", p=64)
        wor_v = wor_d.rearrange("(c p) n -> p c n", p=128)
        worb_v = wor_b.rearrange("(c p) n -> p c n", p=128)
        wout_v = wout_d.rearrange("(c p) n -> p c n", p=128)
        woutb_v = wout_b.rearrange("(c p) n -> p c n", p=128)
        scr_t = {}

        def convert(name, src_v, dst_v, npart, ncols):
            c0 = 0
            while c0 < ncols:
                w = min(512, ncols - c0)
                sl, sl_t = slot_pool.get()
                P.dma("pool", sl[0:npart, :, 0:w], src_v[:, :, c0:c0 + w], writes=[sl_t], st=sl_t)
                t = Tile("scr_%s_%d" % (name, c0))
                scr_t[(name, c0)] = t
                P.dma("sp", dst_v[:, :, c0:c0 + w], sl[0:npart, :, 0:w], reads=[sl_t], writes=[t], st=sl_t)
                c0 += w

        convert("win", win_v, winb_v, 128, IN_TOTAL)
        convert("woa", woa_v, woab_v, 64, D)
        convert("wor", wor_v, worb_v, 128, D)
        convert("wout", wout_v, woutb_v, 128, D)

        def scr_tiles(name, c0, w):
            out = []
            for (n, cc), t in scr_t.items():
                if n == name and cc < c0 + w and c0 < cc + 512:
                    out.append(t)
            return out

        def wload(sl, sl_t, name, view, npart, c0, w, off=0):
            P.dma("sp", sl[0:npart, :, off:off + w], view[:, :, c0:c0 + w], reads=scr_tiles(name, c0, w),
                  writes=[sl_t], st=sl_t)

        KT = sb("KT", [128, 2, L], BF16)
        kt_t = [Tile("kt%d" % i) for i in range(L // T)]
        VA = sb("VA", [128, 32, 2, 128], BF16)
        va_t = [Tile("va%d" % i) for i in range(32)]
        kiT = sb("kiT", [128, L], BF16)
        kit_t = [Tile("kit%d" % i) for i in range(L // T)]
        rxhist = sb("rxhist", [128, 8, 3], F32)
        rxh_t = [Tile("rxh%d" % i) for i in range(8)]
        hst = sb("hst", [128, 8], F32)
        hst_t = [Tile("hst%d" % i) for i in range(8)]
        BDa = sb("BDa", [128, 8, 128], BF16)
        BDx = sb("BDx", [128, 8, 128], BF16)
        bd_t = Tile("bd")
        clam = sb("clam", [128, 16], F32)
        clam_t = Tile("clam")
        I4 = sb("I4", [128, 4, 128], BF16)
        i4_t = Tile("I4")
        ones64 = sb("ones64", [64, 64], BF16)
        ones_t = Tile("ones64")

        P.op("pool", lambda e: e.memset(VA[:, :, :, 64:128], 1.0), writes=va_t)
        P.op("pool", lambda e: e.memset(KT[64:128, :, :], 0.0), writes=kt_t)
        P.op("pool", lambda e: e.memset(kiT[64:128, :], 0.0), writes=kit_t)
        P.op("pool", lambda e: e.memset(rxhist[:], 0.0), writes=rxh_t)
        P.op("pool", lambda e: e.memset(hst[:], 0.0), writes=hst_t)
        P.op("pool", lambda e: e.memset(BDa[:], 0.0), writes=[bd_t])
        P.op("pool", lambda e: e.memset(BDx[:], 0.0), writes=[bd_t])
        P.op("pool", lambda e: e.memset(ones64[:], 1.0 / 64.0), writes=[ones_t])
        for (bd, src) in ((BDa, wa_d), (BDx, wx_d)):
            v = src.rearrange("(m two) d e -> two d m e", two=2)
            P.dma("pool", bd[0:64, :, 0:64], v[0], writes=[bd_t], st=bd_t)
            P.dma("pool", bd[64:128, :, 64:128], v[1], writes=[bd_t], st=bd_t)
        for r in range(4):
            P.op("dve", lambda e, r=r: e.tensor_copy(out=I4[:, r, :], in_=cst[:, 0:128]), reads=[cst_t], writes=[i4_t])
        lam = CV["lam"]
        P.op("act", lambda e: e.activation(out=clam[:, 0:8], in_=cv[:, lam:lam + 8], func=AF.Exp, scale=-1.0),
             reads=[cv_t], writes=[clam_t])
        P.op("act", lambda e: e.activation(out=clam[:, 0:8], in_=clam[:, 0:8], func=AF.Ln, bias=1.0),
             reads=[clam_t], writes=[clam_t])
        P.op("dve", lambda e: e.tensor_scalar(out=clam[:, 8:16], in0=clam[:, 0:8], scalar1=-16.0, scalar2=None, op0=ALU.mult),
             reads=[clam_t], writes=[clam_t])
        P.op("dve", lambda e: e.tensor_scalar(out=clam[:, 0:8], in0=clam[:, 0:8], scalar1=-8.0, scalar2=None, op0=ALU.mult),
             reads=[clam_t], writes=[clam_t])

        big = sb("big", [128, 4096], F32)
        big_t = Tile("big")
        xb = sb("xb", [128, 4, D], F32)
        xb_t = Tile("xb")
        xs = sb("xs1", [128, 4, D], BF16)
        xs_t = Tile("xs1")
        mrgT = xs[:].rearrange("p a (c t) -> p (a c) t", t=T)
        mrg_t = xs_t
        stat = sb("stat1", [128, 8], F32)
        stat_t = Tile("stat1")
        xnTs = [sb("xnT%d" % i, [128, 8, T], BF16) for i in range(2)]
        xnT_ts = [Tile("xnT%d" % i) for i in range(2)]
        qT = sb("qT", [128, 8, T], BF16)
        qT_t = Tile("qT")
        qiT = sb("qiT", [128, 4, T], BF16)
        qiT_t = Tile("qiT")
        P.op("pool", lambda e: e.memset(qT[64:128, :, :], 0.0), writes=[qT_t])
        P.op("pool", lambda e: e.memset(qiT[64:128, :, :], 0.0), writes=[qiT_t])
        wabs = sb("wabs", [128, 4, 4], F32)
        wsgn = sb("wsgn", [128, 4, 4], F32)
        wi_t = [Tile("wi%d" % a) for a in range(4)]
        attnT = sb("attnT", [64, 8, T], BF16)
        attn_t = Tile("attnT")
        rnnT = sb("rnnT", [128, 8, T], BF16)
        rnn_t = Tile("rnnT")
        MBs = [sb("MB%d" % i, [128, 4096], BF16) for i in range(2)]
        mb_ts = [(Tile("MBd%d" % i), Tile("MBa%d" % i)) for i in range(2)]
        bis = sb("bis", [128, 8], F32)
        bis_t = Tile("bis")
        cand = sb("cand", [128, 2], F32)
        cand_t = Tile("cand")
        MS = sb("MS", [128, 16], F32)
        ms_t = Tile("MS")
        stpc = sb("stpc", [128, 16], F32)
        stpc_t = Tile("stpc")
        for k in range(1, 17):
            P.op("pool", lambda e, k=k: e.memset(stpc[:, k - 1:k], 2.0 ** (1 - k)), writes=[stpc_t])
        NF = 8
        ftmp = [sb("ft%d" % i, [128, 3 + T], F32) for i in range(NF)]
        f_pool = Pool([(ftmp[i], Tile("ft%d" % i)) for i in range(NF)])
        NB = 5
        btmp = [sb("bt%d" % i, [128, T], BF16) for i in range(NB)]
        b_pool = Pool([(btmp[i], Tile("bt%d" % i)) for i in range(NB)])
        pb_pool = Pool(list(zip(pbanks[0:4], pb_tiles[0:4])))
        OT = [(pbanks[4], pb_tiles[4]), (pbanks[5], pb_tiles[5])]
        ptr_pool = Pool([(ptr[0], ptr_t[0]), (ptr[1], ptr_t[1])])
        g1 = CV["g1"]
        cw = CV["convw"]

        def proj(pb, pb_t, M, sl, sl_t, c0, xnT, xnT_t):
            for c in range(8):
                P.op("pe", lambda e, c=c: e.matmul(pb[0:M, 0:T], lhsT=sl[:, c, c0:c0 + M], rhs=xnT[:, c, :],
                                                 start=(c == 0), stop=(c == 7)),
                     reads=[xnT_t, sl_t], writes=[pb_t], inc=(c == 7))

        def qknorm(pb, pb_t, out_ap, out_tiles, gcol):
            qf, qf_t = f_pool.get()
            sq, sq_t = b_pool.get()
            P.op("act", lambda e: e.activation(out=qf[0:64, 0:T], in_=pb[0:64, 0:T], func=AF.Copy, scale=cv[0:64, gcol:gcol + 1]),
                 reads=[pb_t, cv_t], writes=[qf_t])
            P.op("act", lambda e: e.activation(out=sq[0:64, :], in_=pb[0:64, 0:T], func=AF.Square), reads=[pb_t], writes=[sq_t])
            p2, p2_t = pb_pool.get()
            P.op("pe", lambda e: e.matmul(p2[0:64, 0:T], lhsT=ones64[:], rhs=sq[0:64, :], start=True, stop=True),
                 reads=[sq_t, ones_t], writes=[p2_t])
            sd, sd_t = f_pool.get()
            P.op("act", lambda e: e.activation(out=sd[0:64, 0:T], in_=p2[0:64, 0:T], func=AF.Ln, bias=EPS), reads=[p2_t], writes=[sd_t])
            P.op("act", lambda e: e.activation(out=sd[0:64, 0:T], in_=sd[0:64, 0:T], func=AF.Exp, scale=-0.5), reads=[sd_t], writes=[sd_t])
            P.op("pool", lambda e: e.tensor_tensor(out=out_ap, in0=qf[0:64, 0:T], in1=sd[0:64, 0:T], op=ALU.mult),
                 reads=[qf_t, sd_t], writes=out_tiles)

        def x_view(s):
            return x_d[s * T:(s + 1) * T, :].rearrange("(a p) d -> p a d", p=128)

        def stage_A(s):
            xnT, xnT_t = xnTs[s % 2], xnT_ts[s % 2]
            P.dma("sp", xb[:], x_view(s), writes=[xb_t], st=xb_t)
            for a in range(4):
                P.op("act", lambda e, a=a: e.activation(out=xs[:, a, :], in_=xb[:, a, :], func=AF.Square,
                                                        accum_out=stat[:, a:a + 1]),
                     reads=[xb_t], writes=[xs_t, stat_t])
            P.op("act", lambda e: e.activation(out=stat[:, 0:4], in_=stat[:, 0:4], func=AF.Ln, scale=1.0 / D, bias=EPS),
                 reads=[stat_t], writes=[stat_t])
            P.op("act", lambda e: e.activation(out=stat[:, 4:8], in_=stat[:, 0:4], func=AF.Exp, scale=-0.5),
                 reads=[stat_t], writes=[stat_t])
            for a in range(4):
                P.op("act", lambda e, a=a: e.activation(out=xs[:, a, :], in_=xb[:, a, :], func=AF.Copy,
                                                        scale=stat[:, 4 + a:5 + a]),
                     reads=[xb_t, stat_t], writes=[xs_t])
            for c in range(8):
                tp, tp_t = ptr_pool.get()
                for a in range(4):
                    P.op("pe", lambda e, a=a, c=c, tp=tp: e.transpose(out=tp[:, a * 128:(a + 1) * 128],
                                                                      in_=xs[:, a, c * 128:(c + 1) * 128],
                                                                      identity=identb[:]),
                         reads=[xs_t, ident_t], writes=[tp_t], inc=(a == 3))
                P.op("act", lambda e, c=c, tp=tp: e.activation(out=xnT[:, c, :], in_=tp[:, 0:T], func=AF.Copy,
                                                               scale=cv[:, g1 + c:g1 + c + 1]),
                     reads=[tp_t, cv_t], writes=[xnT_t])

        def stage_B_idx(s, slk, slk_t, slw, slw_t):
            xnT, xnT_t = xnTs[s % 2], xnT_ts[s % 2]
            tok0 = s * T
            for g in range(2):
                pb, pb_t = pb_pool.get()
                proj(pb, pb_t, 64, slk, slk_t, g * 64, xnT, xnT_t)
                qknorm(pb, pb_t, KT[0:64, g, tok0:tok0 + T], [kt_t[s]], CV["kg"])
            for h in range(4):
                pb, pb_t = pb_pool.get()
                proj(pb, pb_t, 64, slk, slk_t, 256 + h * 64, xnT, xnT_t)
                P.op("act", lambda e, h=h, pb=pb: e.activation(out=qiT[0:64, h, :], in_=pb[0:64, 0:T], func=AF.Copy),
                     reads=[pb_t], writes=[qiT_t])
            pb, pb_t = pb_pool.get()
            proj(pb, pb_t, 64, slw, slw_t, 0, xnT, xnT_t)
            qknorm(pb, pb_t, kiT[0:64, tok0:tok0 + T], [kit_t[s]], CV["kig"])
            for a in range(4):
                j = 4 * s + a
                pb, pb_t = pb_pool.get()
                for c in range(8):
                    P.op("pe", lambda e, c=c, a=a, pb=pb: e.matmul(pb[:, 0:128], lhsT=xnT[:, c, a * 128:(a + 1) * 128],
                                                                 rhs=slk[:, c, 128:256], start=(c == 0), stop=(c == 7)),
                         reads=[xnT_t, slk_t], writes=[pb_t], inc=(c == 7))
                P.op("act", lambda e, j=j, pb=pb: e.activation(out=VA[:, j, :, 0:64],
                                                               in_=pb[:, 0:128].rearrange("p (g d) -> p g d", g=2),
                                                               func=AF.Copy),
                     reads=[pb_t], writes=[va_t[j]])
                pb, pb_t = pb_pool.get()
                for c in range(8):
                    P.op("pe", lambda e, c=c, a=a, pb=pb: e.matmul(pb[:, 0:4], lhsT=xnT[:, c, a * 128:(a + 1) * 128],
                                                                 rhs=slw[:, c, 64:68], start=(c == 0), stop=(c == 7)),
                         reads=[xnT_t, slw_t], writes=[pb_t], inc=(c == 7))
                P.op("act", lambda e, a=a, pb=pb: e.activation(out=wabs[:, a, :], in_=pb[:, 0:4], func=AF.Abs, scale=1.0 / 16.0),
                     reads=[pb_t], writes=[wi_t[a]])
                P.op("act", lambda e, a=a, pb=pb: e.activation(out=wsgn[:, a, :], in_=pb[:, 0:4], func=AF.Sign),
                     reads=[pb_t], writes=[wi_t[a]])

        def stage_B_q(s, slq, slq_t):
            xnT, xnT_t = xnTs[s % 2], xnT_ts[s % 2]
            for h in range(8):
                pb, pb_t = pb_pool.get()
                proj(pb, pb_t, 64, slq, slq_t, h * 64, xnT, xnT_t)
                qknorm(pb, pb_t, qT[0:64, h, :], [qT_t], CV["qg"])

        def att_scores(s, a):
            i = 4 * s + a
            n = 128 * (i + 1)
            qs = slice(a * 128, (a + 1) * 128)
            nch = (n + 511) // 512
            for c in range(nch):
                w = min(512, n - 512 * c)
                ks = slice(512 * c, 512 * c + w)
                for h in range(4):
                    pb, pb_t = pb_pool.get()
                    P.op("pe", lambda e, h=h, pb=pb, w=w, ks=ks: e.matmul(pb[:, 0:w], lhsT=qiT[:, h, qs], rhs=kiT[:, ks],
                                                                         start=True, stop=True),
                         reads=[qiT_t] + kit_t[0:s + 1], writes=[pb_t])
                    r, r_t = f_pool.get()
                    P.op("act", lambda e, h=h, pb=pb, r=r, w=w: e.activation(out=r[:, 0:w], in_=pb[:, 0:w], func=AF.Relu,
                                                                           scale=wabs[:, a, h:h + 1]),
                         reads=[pb_t, wi_t[a]], writes=[r_t])
                    if h == 0:
                        P.op("dve", lambda e, r=r, w=w, ks=ks: e.tensor_scalar(out=big[:, ks], in0=r[:, 0:w],
                                                                              scalar1=wsgn[:, a, 0:1], scalar2=None,
                                                                              op0=ALU.mult),
                             reads=[r_t, wi_t[a]], writes=[big_t])
                    else:
                        P.op("dve", lambda e, h=h, r=r, w=w, ks=ks: e.scalar_tensor_tensor(
                            out=big[:, ks], in0=r[:, 0:w], scalar=wsgn[:, a, h:h + 1], in1=big[:, ks],
                            op0=ALU.mult, op1=ALU.add),
                            reads=[r_t, wi_t[a]], writes=[big_t])
            if i >= 2:
                P.op("dve", lambda e: e.tensor_reduce(out=bis[:, 0:1], in_=big[:, 0:n], axis=AX.X, op=ALU.max,
                                                      apply_absolute_value=True),
                     reads=[big_t], writes=[bis_t])
                P.op("dve", lambda e: e.tensor_scalar(out=bis[:, 1:2], in0=bis[:, 0:1], scalar1=-1.0, scalar2=None, op0=ALU.mult),
                     reads=[bis_t], writes=[bis_t])
            else:
                P.op("dve", lambda e: e.memset(bis[:, 1:2], -1e29), writes=[bis_t])
            P.op("dve", lambda e: e.tensor_tensor(out=big[:, n - 128:n], in0=big[:, n - 128:n], in1=cst[:, 128:256], op=ALU.add),
                 reads=[cst_t, big_t], writes=[big_t])

        def att_bisect(s, a):
            i = 4 * s + a
            n = 128 * (i + 1)
            MB = MBs[i % 2]
            mbD_t, mbA_t = mb_ts[i % 2]
            nit = NIT - (2 if n <= 1024 else 1 if n <= 2048 else 0)
            if i >= 2:
                P.op("dve", lambda e: e.tensor_scalar(out=MS[:, 0:nit], in0=stpc[:, 0:nit], scalar1=bis[:, 0:1], scalar2=None, op0=ALU.mult),
                     reads=[bis_t, stpc_t], writes=[ms_t])
                P.op("dve", lambda e: e.memset(cand[:, 0:1], 0.0), writes=[cand_t])
                for k in range(1, nit + 1):
                    P.op("dve", lambda e: e.tensor_scalar(out=MB[:, 0:n], in0=big[:, 0:n], scalar1=cand[:, 0:1], scalar2=None,
                                                          op0=ALU.is_ge, op1=ALU.add, accum_out=bis[:, 3:4]),
                         reads=[big_t, cand_t], writes=[mbD_t, bis_t])
                    P.op("dve", lambda e: e.tensor_scalar(out=bis[:, 4:5], in0=bis[:, 3:4], scalar1=float(TOPK) - 0.5, scalar2=-0.5,
                                                          op0=ALU.is_ge, op1=ALU.add),
                         reads=[bis_t], writes=[bis_t])
                    P.op("dve", lambda e, k=k: e.scalar_tensor_tensor(out=cand[:, 0:1], in0=bis[:, 4:5], scalar=MS[:, k - 1:k], in1=cand[:, 0:1],
                                                                      op0=ALU.mult, op1=ALU.add),
                         reads=[bis_t, ms_t, cand_t], writes=[cand_t])
                P.op("dve", lambda e: e.scalar_tensor_tensor(out=bis[:, 1:2], in0=MS[:, nit - 1:nit], scalar=-0.5, in1=cand[:, 0:1],
                                                             op0=ALU.mult, op1=ALU.add),
                     reads=[ms_t, cand_t], writes=[bis_t])
            P.op("dve", lambda e: e.tensor_scalar(out=MB[:, 0:n], in0=big[:, 0:n], scalar1=bis[:, 1:2], scalar2=NEG,
                                                  op0=ALU.is_lt, op1=ALU.mult),
                 reads=[big_t, bis_t], writes=[mbD_t, mbA_t])

        def att_main(s, a):
            i = 4 * s + a
            qs = slice(a * 128, (a + 1) * 128)
            MB = MBs[i % 2]
            mb_tt = list(mb_ts[i % 2])
            items = [(j, g) for j in range(i + 1) for g in range(2)]

            def st_exp(j, g):
                kb = slice(128 * j, 128 * j + 128)
                pb, pb_t = pb_pool.get()
                P.op("pe", lambda e: e.matmul(pb[:, :], lhsT=KT[:, g, kb], rhs=qT[:, 4 * g:4 * g + 4, qs],
                                              start=True, stop=False),
                     reads=[qT_t] + kt_t[0:s + 1], writes=[pb_t], inc=False)
                P.op("pe", lambda e: e.matmul(pb[:, :], lhsT=MB[:, kb], rhs=I4[:].rearrange("p r q -> p (r q)"),
                                              start=False, stop=True),
                     reads=mb_tt + [i4_t], writes=[pb_t])
                pt, pt_t = b_pool.get()
                P.op("act", lambda e: e.activation(out=pt[:], in_=pb[:, :], func=AF.Exp, scale=0.125),
                     reads=[pb_t], writes=[pt_t])
                return pt, pt_t

            def pv(j, g, pt, pt_t):
                ot, ot_t = OT[g]
                P.op("pe", lambda e: e.matmul(ot[:, :], lhsT=VA[:, j, g, :], rhs=pt[:],
                                              start=(j == 0), stop=(j == i)),
                     reads=[pt_t, va_t[j]], writes=[ot_t], inc=(j == i))

            LOOK = 2
            pend = []
            for k, (j, g) in enumerate(items):
                pend.append((j, g) + st_exp(j, g))
                if len(pend) > LOOK:
                    pv(*pend.pop(0))
            while pend:
                pv(*pend.pop(0))
            for g in range(2):
                ot, ot_t = OT[g]
                rc, rc_t = f_pool.get()
                P.op("act", lambda e, ot=ot, rc=rc: e.activation(out=rc[0:64, 0:T], in_=ot[64:128, :], func=AF.Ln), reads=[ot_t], writes=[rc_t])
                P.op("act", lambda e, rc=rc: e.activation(out=rc[0:64, 0:T], in_=rc[0:64, 0:T], func=AF.Exp, scale=-1.0), reads=[rc_t], writes=[rc_t])
                P.op("dve", lambda e, g=g, ot=ot, rc=rc: e.tensor_tensor(
                    out=attnT[:, 4 * g:4 * g + 4, qs], in0=ot[0:64, :].rearrange("p (r q) -> p r q", r=4),
                    in1=rc[0:64, 0:T].rearrange("p (r q) -> p r q", r=4), op=ALU.mult),
                    reads=[ot_t, rc_t], writes=[attn_t])

        rnn_state = {}
        xc_ded = [(sb("xcd%d" % i, [128, T], F32), Tile("xcd%d" % i)) for i in range(2)]
        xcb_ded = [(sb("xcbd%d" % i, [128, T], BF16), Tile("xcbd%d" % i)) for i in range(2)]

        def rnn_part1(s, m, slr, slr_t):
            xnT, xnT_t = xnTs[s % 2], xnT_ts[s % 2]
            mm = m % 4
            pb, pb_t = pb_pool.get()
            proj(pb, pb_t, 128, slr, slr_t, mm * 128, xnT, xnT_t)
            rxh, rxh_tt = f_pool.get()
            P.op("pool", lambda e: e.tensor_copy(out=rxh[:, 0:3], in_=rxhist[:, m, :]), reads=[rxh_t[m]], writes=[rxh_tt])
            P.op("act", lambda e: e.activation(out=rxh[:, 3:3 + T], in_=pb[:, :], func=AF.Copy), reads=[pb_t], writes=[rxh_tt])
            P.op("pool", lambda e: e.tensor_copy(out=rxhist[:, m, :], in_=rxh[:, T:T + 3]), reads=[rxh_tt], writes=[rxh_t[m]])
            xc, xc_t = xc_ded[m % 2]
            P.op("dve", lambda e: e.tensor_scalar(
                out=xc[:, 0:T], in0=rxh[:, 3:3 + T], scalar1=cv[:, cw + 3 * 8 + m:cw + 3 * 8 + m + 1],
                scalar2=cv[:, CV["convb"] + m:CV["convb"] + m + 1], op0=ALU.mult, op1=ALU.add),
                reads=[rxh_tt, cv_t], writes=[xc_t])
            for jj in (2, 1, 0):
                P.op("dve", lambda e, jj=jj: e.scalar_tensor_tensor(
                    out=xc[:, 0:T], in0=rxh[:, jj:jj + T], scalar=cv[:, cw + jj * 8 + m:cw + jj * 8 + m + 1],
                    in1=xc[:, 0:T], op0=ALU.mult, op1=ALU.add),
                    reads=[rxh_tt, cv_t], writes=[xc_t])
            xcb, xcb_t = xcb_ded[m % 2]
            P.op("act", lambda e: e.activation(out=xcb[:], in_=xc[:, 0:T], func=AF.Copy), reads=[xc_t], writes=[xcb_t])
            rnn_state[m] = (xc, xc_t, xcb, xcb_t)

        def rnn_part2(s, m, slg, slg_t):
            xnT, xnT_t = xnTs[s % 2], xnT_ts[s % 2]
            mm = m % 4
            xc, xc_t, xcb, xcb_t = rnn_state.pop(m)
            pr, pr_t = pb_pool.get()
            P.op("pe", lambda e: e.matmul(pr[:, :], lhsT=BDa[:, m, :], rhs=xcb[:], start=True, stop=True),
                 reads=[xcb_t, bd_t], writes=[pr_t])
            pi, pi_t = pb_pool.get()
            P.op("pe", lambda e: e.matmul(pi[:, :], lhsT=BDx[:, m, :], rhs=xcb[:], start=True, stop=True),
                 reads=[xcb_t, bd_t], writes=[pi_t])
            rr, rr_t = f_pool.get()
            P.op("act", lambda e: e.activation(out=rr[:, 0:T], in_=pr[:, :], func=AF.Sigmoid,
                                               bias=cv[:, CV["ba"] + m:CV["ba"] + m + 1]),
                 reads=[pr_t, cv_t], writes=[rr_t])
            ii, ii_t = f_pool.get()
            P.op("act", lambda e: e.activation(out=ii[:, 0:T], in_=pi[:, :], func=AF.Sigmoid,
                                               bias=cv[:, CV["bx"] + m:CV["bx"] + m + 1]),
                 reads=[pi_t, cv_t], writes=[ii_t])
            aa, aa_t = f_pool.get()
            P.op("act", lambda e: e.activation(out=aa[:, 0:T], in_=rr[:, 0:T], func=AF.Exp, scale=clam[:, m:m + 1]),
                 reads=[rr_t, clam_t], writes=[aa_t])
            P.op("act", lambda e: e.activation(out=rr[:, 0:T], in_=rr[:, 0:T], func=AF.Exp, scale=clam[:, 8 + m:9 + m]),
                 reads=[rr_t, clam_t], writes=[rr_t])
            P.op("act", lambda e: e.activation(out=rr[:, 0:T], in_=rr[:, 0:T], func=AF.Relu, scale=-1.0, bias=1.0),
                 reads=[rr_t], writes=[rr_t])
            P.op("act", lambda e: e.activation(out=rr[:, 0:T], in_=rr[:, 0:T], func=AF.Sqrt, bias=1e-30),
                 reads=[rr_t], writes=[rr_t])
            P.op("dve", lambda e: e.tensor_tensor(out=ii[:, 0:T], in0=ii[:, 0:T], in1=xc[:, 0:T], op=ALU.mult),
                 reads=[ii_t, xc_t], writes=[ii_t])
            P.op("dve", lambda e: e.tensor_tensor(out=ii[:, 0:T], in0=ii[:, 0:T], in1=rr[:, 0:T], op=ALU.mult),
                 reads=[ii_t, rr_t], writes=[ii_t])
            hh, hh_t = f_pool.get()
            P.op("dve", lambda e: e.tensor_tensor_scan(
                out=hh[:, 0:T], data0=aa[:, 0:T], data1=ii[:, 0:T], initial=hst[:, m:m + 1], op0=ALU.mult, op1=ALU.add),
                reads=[aa_t, ii_t, hst_t[m]], writes=[hh_t])
            P.op("pool", lambda e: e.tensor_copy(out=hst[:, m:m + 1], in_=hh[:, T - 1:T]), reads=[hh_t], writes=[hst_t[m]])
            pg, pg_t = pb_pool.get()
            proj(pg, pg_t, 128, slg, slg_t, mm * 128, xnT, xnT_t)
            gl, gl_t = f_pool.get()
            P.op("act", lambda e: e.activation(out=gl[:, 0:T], in_=pg[:, :], func=AF.Gelu_apprx_tanh), reads=[pg_t], writes=[gl_t])
            P.op("dve", lambda e: e.tensor_tensor(out=rnnT[:, m, :], in0=hh[:, 0:T], in1=gl[:, 0:T], op=ALU.mult),
                 reads=[hh_t, gl_t], writes=[rnn_t])

        def merge_pair(s, fp, sg_, sg_t, so_, so_t):
            xnT, xnT_t = xnTs[s % 2], xnT_ts[s % 2]
            for ff in range(2):
                f = 2 * fp + ff
                pga, pga_t = pb_pool.get()
                proj(pga, pga_t, 128, sg_, sg_t, ff * 128, xnT, xnT_t)
                sa, sa_t = f_pool.get()
                P.op("act", lambda e, pga=pga, sa=sa: e.activation(out=sa[:, 0:T], in_=pga[:, :], func=AF.Sigmoid),
                     reads=[pga_t], writes=[sa_t])
                pgb, pgb_t = pb_pool.get()
                proj(pgb, pgb_t, 128, sg_, sg_t, 256 + ff * 128, xnT, xnT_t)
                sbb, sbb_t = f_pool.get()
                P.op("act", lambda e, pgb=pgb, sbb=sbb: e.activation(out=sbb[:, 0:T], in_=pgb[:, :], func=AF.Sigmoid),
                     reads=[pgb_t], writes=[sbb_t])
                pa, pa_t = pb_pool.get()
                for h in range(8):
                    P.op("pe", lambda e, h=h, ff=ff, pa=pa: e.matmul(pa[:, :], lhsT=so_[0:64, h, ff * 128:(ff + 1) * 128],
                                                                    rhs=attnT[:, h, :], start=(h == 0), stop=(h == 7)),
                         reads=[attn_t, so_t], writes=[pa_t], inc=(h == 7))
                P.op("dve", lambda e, sa=sa, pa=pa: e.tensor_tensor(out=sa[:, 0:T], in0=sa[:, 0:T], in1=pa[:, :], op=ALU.mult),
                     reads=[sa_t, pa_t], writes=[sa_t])
                pbm, pbm_t = pb_pool.get()
                for c in range(8):
                    P.op("pe", lambda e, c=c, ff=ff, pbm=pbm: e.matmul(pbm[:, :], lhsT=so_[:, c, 256 + ff * 128:256 + (ff + 1) * 128],
                                                                      rhs=rnnT[:, c, :], start=(c == 0), stop=(c == 7)),
                         reads=[rnn_t, so_t], writes=[pbm_t], inc=(c == 7))
                P.op("dve", lambda e, sbb=sbb, pbm=pbm: e.tensor_tensor(out=sbb[:, 0:T], in0=sbb[:, 0:T], in1=pbm[:, :], op=ALU.mult),
                     reads=[sbb_t, pbm_t], writes=[sbb_t])
                P.op("dve", lambda e, f=f, sa=sa, sbb=sbb: e.tensor_tensor(out=mrgT[:, f, :], in0=sa[:, 0:T], in1=sbb[:, 0:T], op=ALU.add),
                     reads=[sa_t, sbb_t], writes=[mrg_t])

        def outproj_half(s, nn, swo, swo_t):
            for a in range(4):
                pb, pb_t = pb_pool.get()
                for f in range(8):
                    P.op("pe", lambda e, a=a, f=f, pb=pb: e.matmul(pb[:, :], lhsT=mrgT[:, f, a * 128:(a + 1) * 128],
                                                                  rhs=swo[:, f, :], start=(f == 0), stop=(f == 7)),
                         reads=[mrg_t, swo_t], writes=[pb_t], inc=(f == 7))
                P.op("dve", lambda e, a=a, pb=pb: e.tensor_tensor(out=xb[:, a, nn * 512:(nn + 1) * 512], in0=pb[:, :],
                                                                 in1=xb[:, a, nn * 512:(nn + 1) * 512], op=ALU.add),
                     reads=[pb_t, xb_t], writes=[xb_t])

        slot_free = list(slot_pool.items)

        class _SP:
            @staticmethod
            def get():
                assert slot_free, "no free weight slot"
                return slot_free.pop(0)

        def release(w):
            for k in range(0, len(w), 2):
                slot_free.append((w[k], w[k + 1]))

        def L_rnn(half):
            def f():
                a_ = _SP.get(); wload(a_[0], a_[1], "win", winb_v, 128, 1092 + 512 * half, 512)
                b_ = _SP.get(); wload(b_[0], b_[1], "win", winb_v, 128, 2116 + 512 * half, 512)
                return a_ + b_
            return f

        def L_idx():
            a_ = _SP.get(); wload(a_[0], a_[1], "win", winb_v, 128, 512, 512)
            b_ = _SP.get(); wload(b_[0], b_[1], "win", winb_v, 128, 1024, 68)
            return a_ + b_

        def L_q():
            a_ = _SP.get(); wload(a_[0], a_[1], "win", winb_v, 128, 0, 512)
            return a_

        def L_merge(fp):
            def f():
                a_ = _SP.get()
                wload(a_[0], a_[1], "win", winb_v, 128, 3140 + 256 * fp, 256, off=0)
                wload(a_[0], a_[1], "win", winb_v, 128, 4164 + 256 * fp, 256, off=256)
                b_ = _SP.get()
                wload(b_[0], b_[1], "woa", woab_v, 64, 256 * fp, 256, off=0)
                wload(b_[0], b_[1], "wor", worb_v, 128, 256 * fp, 256, off=256)
                return a_ + b_
            return f

        def L_out(nn):
            def f():
                a_ = _SP.get(); wload(a_[0], a_[1], "wout", woutb_v, 128, 512 * nn, 512)
                return a_
            return f

        stage_A(0)
        w_idx = L_idx()
        w_q = L_q()
        stage_B_idx(0, *w_idx)
        release(w_idx)
        stage_B_q(0, *w_q)
        release(w_q)
        w_r0 = L_rnn(0)()
        att_scores(0, 0)
        att_bisect(0, 0)
        for s in range(NS):
            last = (s == NS - 1)
            w_r1 = None
            for a in range(4):
                if a == 0:
                    w_r1 = L_rnn(1)()
                if a == 2 and not last:
                    w_idx = L_idx()
                if a == 3 and not last:
                    w_q = L_q()
                wr = w_r0 if a < 2 else w_r1
                for m in (2 * a, 2 * a + 1):
                    rnn_part1(s, m, wr[0], wr[1])
                if a < 3:
                    att_scores(s, a + 1)
                    att_bisect(s, a + 1)
                if a == 2 and not last:
                    stage_A(s + 1)
                    stage_B_idx(s + 1, *w_idx)
                    release(w_idx)
                if a == 3 and not last:
                    att_scores(s + 1, 0)
                    att_bisect(s + 1, 0)
                att_main(s, a)
                for m in (2 * a, 2 * a + 1):
                    rnn_part2(s, m, wr[2], wr[3])
                if a == 1:
                    release(w_r0)
                if a == 3:
                    release(w_r1)
            w_m0 = L_merge(0)()
            if not last:
                stage_B_q(s + 1, *w_q)
                release(w_q)
            w_m1 = L_merge(1)()
            merge_pair(s, 0, *w_m0)
            release(w_m0)
            w_m2 = L_merge(2)()
            merge_pair(s, 1, *w_m1)
            release(w_m1)
            w_m3 = L_merge(3)()
            merge_pair(s, 2, *w_m2)
            release(w_m2)
            w_o0 = L_out(0)()
            merge_pair(s, 3, *w_m3)
            release(w_m3)
            w_o1 = L_out(1)()
            P.dma("sp", xb[:], x_view(s), writes=[xb_t], st=xb_t)
            outproj_half(s, 0, *w_o0)
            release(w_o0)
            if not last:
                w_r0 = L_rnn(0)()
            outproj_half(s, 1, *w_o1)
            release(w_o1)
            P.dma("sp", x1_d[s * T:(s + 1) * T, :].rearrange("(a p) d -> p a d", p=128), xb[:],
                  reads=[xb_t], writes=[x1_t[s]], st=xb_t)
        P.wait_all("sp", [xb_t] + x1_t)


def phase2(nc, P, es2, sb_outer, x1_t, x1_d, out_d, wup_d, wdn_d, cv, cv_t, identb, ident_t, pbanks, pb_tiles, ptr, ptr_t):
    T2 = 256
    NS2 = L // T2
    with ExitStack() as es:
        def sb(name, shape, dt):
            return es.enter_context(nc.sbuf_tensor(name, shape, dt))

        wup = sb("wup", [128, 8, 2 * DFF], BF16)
        wdn = sb("wdn", [128, NFF, D], BF16)
        wup_t = [Tile("wup%d" % i) for i in range(11)]
        wdn_t = [Tile("wdn%d" % i) for i in range(2)]
        wup_v = wup_d.rearrange("(c p) n -> p c n", p=128)
        wdn_v = wdn_d.rearrange("(f p) n -> p f n", p=128)
        for i in range(11):
            P.dma("pool", wup[:, :, i * 512:(i + 1) * 512], wup_v[:, :, i * 512:(i + 1) * 512],
                  writes=[wup_t[i]], st=wup_t[i])
        for i in range(2):
            P.dma("pool", wdn[:, :, i * 512:(i + 1) * 512], wdn_v[:, :, i * 512:(i + 1) * 512],
                  writes=[wdn_t[i]], st=wdn_t[i])

        hist = sb("hist", [128, 2 * NFF, 2], F32)
        hist_t = [Tile("hist%d" % i) for i in range(2 * NFF)]
        hist_all = Tile("hist_all")
        P.op("pool", lambda e: e.memset(hist[:], 0.0), writes=hist_t)

        xts = [sb("xt%d" % i, [128, 2, D], F32) for i in range(3)]
        xt_pool = Pool([(xts[i], Tile("xt%d" % i)) for i in range(3)])
        xs = sb("xs", [128, 2, D], BF16)
        xs_t = Tile("xs")
        stat = sb("stat2", [128, 8], F32)
        stat_pool = Pool([(stat[:, 0:2], stat[:, 2:4], Tile("st2a")), (stat[:, 4:6], stat[:, 6:8], Tile("st2b"))])
        xnTs = [sb("xn2T%d" % i, [128, 8, T2], BF16) for i in range(2)]
        xnT_ts = [Tile("xn2T%d" % i) for i in range(2)]
        hTs = [sb("hT%d" % i, [128, NFF, T2], BF16) for i in range(2)]
        hT_ts = [Tile("hT%d" % i) for i in range(2)]
        upbs = [sb("upb%d" % i, [128, 2 + T2], F32) for i in range(6)]
        upb_pool = Pool([(upbs[i], Tile("upb%d" % i)) for i in range(6)])
        us = [sb("u%d" % i, [128, T2], F32) for i in range(6)]
        u_pool = Pool([(us[i], Tile("u%d" % i)) for i in range(6)])
        sgs = [sb("sg%d" % i, [128, T2], F32) for i in range(3)]
        sg_pool = Pool([(sgs[i], Tile("sg%d" % i)) for i in range(3)])
        pb_pool = Pool(list(zip(pbanks[0:5], pb_tiles[0:5])))
        pdn, pdn_t = pbanks[5], pb_tiles[5]
        ptr_pool = Pool([(ptr[0], ptr_t[0]), (ptr[1], ptr_t[1])])
        fcw = CV["fcw"]
        fcb = CV["fcb"]
        g2 = CV["g2"]
        NS2 = DBG.get('ns2', NS2)

        def load_x(s):
            xt, xt_t = xt_pool.get()
            P.dma("sp", xt[:], x1_d[s * T2:(s + 1) * T2, :].rearrange("(a p) d -> p a d", p=128),
                  reads=([x1_t[s // 2]] if x1_t is not None else []), writes=[xt_t], st=xt_t)
            return xt, xt_t

        def stage_A2(s, xt, xt_t):
            xnT, xnT_t = xnTs[s % 2], xnT_ts[s % 2]
            ss, rs, st_t = stat_pool.get()
            for a in range(2):
                P.op("act", lambda e, a=a: e.activation(out=xs[:, a, :], in_=xt[:, a, :], func=AF.Square,
                                                        accum_out=ss[:, a:a + 1]),
                     reads=[xt_t], writes=[xs_t, st_t])
            P.op("act", lambda e: e.activation(out=ss, in_=ss, func=AF.Ln, scale=1.0 / D, bias=EPS),
                 reads=[st_t], writes=[st_t])
            P.op("act", lambda e: e.activation(out=rs, in_=ss, func=AF.Exp, scale=-0.5), reads=[st_t], writes=[st_t])
            for a in range(2):
                P.op("act", lambda e, a=a: e.activation(out=xs[:, a, :], in_=xt[:, a, :], func=AF.Copy,
                                                        scale=rs[:, a:a + 1]),
                     reads=[xt_t, st_t], writes=[xs_t])
            for c in range(8):
                tp, tp_t = ptr_pool.get()
                for a in range(2):
                    P.op("pe", lambda e, a=a, c=c, tp=tp: e.transpose(out=tp[:, a * 128:(a + 1) * 128],
                                                                      in_=xs[:, a, c * 128:(c + 1) * 128],
                                                                      identity=identb[:]),
                         reads=[xs_t, ident_t], writes=[tp_t], inc=(a == 1))
                P.op("act", lambda e, c=c, tp=tp: e.activation(out=xnT[:, c, :], in_=tp[:, 0:T2], func=AF.Copy,
                                                               scale=cv[:, g2 + c:g2 + c + 1]),
                     reads=[tp_t, cv_t], writes=[xnT_t])

        def down_ops(s, xt, xt_t):
            hT, hT_t = hTs[s % 2], hT_ts[s % 2]
            for a in range(2):
                for n in range(2):
                    for f in range(NFF):
                        P.op("pe", lambda e, a=a, n=n, f=f: e.matmul(pdn[:, :], lhsT=hT[:, f, a * 128:(a + 1) * 128],
                                                                   rhs=wdn[:, f, n * 512:(n + 1) * 512],
                                                                   start=(f == 0), stop=(f == NFF - 1)),
                             reads=[hT_t, wdn_t[n]], writes=[pdn_t], inc=(f == NFF - 1))
                        if f == NFF - 1:
                            P.op("dve", lambda e, a=a, n=n: e.tensor_tensor(out=xt[:, a, n * 512:(n + 1) * 512], in0=pdn[:, :],
                                                                           in1=xt[:, a, n * 512:(n + 1) * 512], op=ALU.add),
                                 reads=[pdn_t, xt_t], writes=[xt_t])
                        yield
            P.dma("sp", out_d[s * T2:(s + 1) * T2, :].rearrange("(a p) d -> p a d", p=128), xt[:],
                  reads=[xt_t], st=xt_t)
            yield

        def pull(gen, k):
            if gen is None:
                return None
            for _ in range(k):
                try:
                    next(gen)
                except StopIteration:
                    return None
            return gen

        cur = load_x(0)
        stage_A2(0, *cur)
        dgen = None
        for s in range(NS2):
            xt, xt_t = cur
            xnT, xnT_t = xnTs[s % 2], xnT_ts[s % 2]
            hT, hT_t = hTs[s % 2], hT_ts[s % 2]
            if s + 1 < NS2:
                nxt = load_x(s + 1)
            for f in range(NFF):
                uu = []
                for ff in (f, NFF + f):
                    pb, pb_t = pb_pool.get()
                    for c in range(8):
                        P.op("pe", lambda e, c=c, ff=ff, pb=pb: e.matmul(pb[:, 0:T2], lhsT=wup[:, c, ff * 128:(ff + 1) * 128],
                                                                       rhs=xnT[:, c, :], start=(c == 0), stop=(c == 7)),
                             reads=[xnT_t, wup_t[ff // 4]], writes=[pb_t], inc=(c == 7))
                    upb, upb_t = upb_pool.get()
                    P.op("pool", lambda e, upb=upb, ff=ff: e.tensor_copy(out=upb[:, 0:2], in_=hist[:, ff, :]),
                         reads=[hist_t[ff]], writes=[upb_t])
                    P.op("act", lambda e, upb=upb, pb=pb: e.activation(out=upb[:, 2:2 + T2], in_=pb[:, 0:T2], func=AF.Copy),
                         reads=[pb_t], writes=[upb_t])
                    P.op("pool", lambda e, upb=upb, ff=ff: e.tensor_copy(out=hist[:, ff, :], in_=upb[:, T2:T2 + 2]),
                         reads=[upb_t], writes=[hist_t[ff]])
                    u, u_t = u_pool.get()
                    P.op("act", lambda e, u=u, pb=pb, ff=ff: e.activation(
                        out=u[:], in_=pb[:, 0:T2], func=AF.Identity, scale=cv[:, fcw + 2 * 44 + ff:fcw + 2 * 44 + ff + 1],
                        bias=cv[:, fcb + ff:fcb + ff + 1]),
                        reads=[pb_t, cv_t], writes=[u_t])
                    for j in (1, 0):
                        P.op("dve", lambda e, u=u, upb=upb, ff=ff, j=j: e.scalar_tensor_tensor(
                            out=u[:], in0=upb[:, j:j + T2], scalar=cv[:, fcw + j * 44 + ff:fcw + j * 44 + ff + 1],
                            in1=u[:], op0=ALU.mult, op1=ALU.add),
                            reads=[upb_t, cv_t], writes=[u_t])
                    uu.append((u, u_t))
                sg, sg_t = sg_pool.get()
                P.op("act", lambda e, sg=sg, u=uu[0][0]: e.activation(out=sg[:], in_=u[:], func=AF.Silu),
                     reads=[uu[0][1]], writes=[sg_t])
                P.op("dve", lambda e, sg=sg, u=uu[1][0], f=f: e.tensor_tensor(out=hT[:, f, :], in0=sg[:], in1=u[:], op=ALU.mult),
                     reads=[sg_t, uu[1][1]], writes=[hT_t])
                dgen = pull(dgen, 4 if f < NFF - 1 else 1000)
                if f == 8 and s + 1 < NS2:
                    stage_A2(s + 1, *nxt)
            dgen = down_ops(s, xt, xt_t)
            if s + 1 < NS2:
                cur = nxt
        pull(dgen, 1000)
        P.wait_all("sp", [t for _, t in xt_pool.items])


def pack_cvec(inp):
    cvv = np.zeros((128, NV), np.float32)

    def put(name, vec, width):
        cvv[:, CV[name]:CV[name] + width] = np.asarray(vec, np.float32).reshape(width, 128).T

    put("g1", inp["norm1_g"][0], 8)
    put("g2", inp["norm2_g"][0], 8)
    put("convw", inp["conv_w"][0].reshape(-1), 32)
    put("convb", inp["conv_b"][0], 8)
    put("ba", inp["rg_ba"][0], 8)
    put("bx", inp["rg_bx"][0], 8)
    put("lam", inp["rg_lambda"][0], 8)
    put("fcw", inp["ffn_conv_w"][0].reshape(-1), 132)
    put("fcb", inp["ffn_conv_b"][0], 44)
    cvv[0:64, CV["qg"]] = np.asarray(inp["q_norm_g"][0], np.float32)
    cvv[0:64, CV["kg"]] = np.asarray(inp["k_norm_g"][0], np.float32)
    cvv[0:64, CV["kig"]] = np.asarray(inp["kidx_norm_g"][0], np.float32)
    return cvv


def make_in_maps(inp):
    cvv = pack_cvec(inp)
    cst = np.zeros((128, 256), np.float32)
    cst[:, 0:128] = np.eye(128, dtype=np.float32)
    cst[:, 128:256] = np.where(np.arange(128)[None, :] <= np.arange(128)[:, None], 0.0, -1e30)
    shared = {
        "cvec": cvv,
        "consts": cst,
        "w_in": np.ascontiguousarray(inp["w_in"][0], dtype=np.float32),
        "rg_wa": np.ascontiguousarray(inp["rg_wa"][0], dtype=np.float32),
        "rg_wx": np.ascontiguousarray(inp["rg_wx"][0], dtype=np.float32),
        "w_o_attn": np.ascontiguousarray(inp["w_o_attn"][0], dtype=np.float32),
        "w_o_rnn": np.ascontiguousarray(inp["w_o_rnn"][0], dtype=np.float32),
        "w_out": np.ascontiguousarray(inp["w_out"][0], dtype=np.float32),
        "w_up": np.ascontiguousarray(inp["w_up"][0], dtype=np.float32),
        "w_down": np.ascontiguousarray(inp["w_down"][0], dtype=np.float32),
    }
    x = np.asarray(inp["x"], np.float32)
    maps = []
    for b in range(NCORES):
        m = dict(shared)
        m["x"] = np.ascontiguousarray(x[b])
        maps.append(m)
    return maps


def kernel(**inputs):
    nc = build_nc()
    in_maps = make_in_maps(inputs)
    res = run_bass_kernel_spmd(nc, in_maps, core_ids=list(range(NCORES)))
    return np.stack([np.asarray(r["out"], np.float32) for r in res.results], axis=0)
```

```python
from contextlib import ExitStack

import numpy as np
import concourse.bass as bass
import concourse.mybir as mybir
from concourse.bass_utils import run_bass_kernel_spmd

F32 = mybir.dt.float32
BF16 = mybir.dt.bfloat16
AF = mybir.ActivationFunctionType
ALU = mybir.AluOpType
AX = mybir.AxisListType

L = 4096
D = 1024
NCORES = 8
DFF = 2816
NFF = DFF // 128
IN_TOTAL = 5188
EPS = 1e-6
TOPK = 256
NEG = -30000.0

CV = {}
_o = 0
for _n, _w in (("g1", 8), ("g2", 8), ("convw", 32), ("convb", 8), ("ba", 8), ("bx", 8),
               ("lam", 8), ("fcw", 132), ("fcb", 44), ("qg", 1), ("kg", 1), ("kig", 1)):
    CV[_n] = _o
    _o += _w
NV = _o


class Ev:
    __slots__ = ("sem", "val", "eng")

    def __init__(self, sem, val, eng):
        self.sem, self.val, self.eng = sem, val, eng


class Tile:
    __slots__ = ("name", "w", "r", "dsem", "dcnt")

    def __init__(self, name):
        self.name = name
        self.w = None
        self.r = []
        self.dsem = None
        self.dcnt = 0


class Pool:
    def __init__(self, items):
        self.items = items
        self.i = 0

    def get(self):
        it = self.items[self.i % len(self.items)]
        self.i += 1
        return it


class Prog:
    def __init__(self, nc, es):
        self.nc = nc
        self.es = es
        self.eng = {"pe": nc.tensor, "act": nc.scalar, "dve": nc.vector, "pool": nc.gpsimd, "sp": nc.sync}
        self.sem = {k: es.enter_context(nc.semaphore("sem_" + k)) for k in self.eng}
        self.cnt = {k: 0 for k in self.eng}
        self.waited = {k: {} for k in self.eng}
        self.pending = {k: [] for k in self.eng}
        self.nsem = 0
        self.nwait = 0
        self.dsems = []

    def _wait(self, ek, deps):
        best = {}
        for ev in deps:
            if ev is None:
                continue
            if ev.eng == "pe" and ek == "pe":
                continue
            if ev.val is None:
                raise RuntimeError("dependency on unresolved (non-inc) instruction")
            key = id(ev.sem)
            if key not in best or best[key].val < ev.val:
                best[key] = ev
        w = self.waited[ek]
        for key, ev in best.items():
            if w.get(key, 0) < ev.val:
                self.eng[ek].wait_ge(ev.sem, ev.val)
                w[key] = ev.val
                self.nwait += 1

    def _record(self, ev, reads, writes):
        for t in writes:
            t.w = ev
            t.r = []
        for t in reads:
            t.r = [e for e in t.r if e.sem is not ev.sem] + [ev]

    def op(self, ek, fn, reads=(), writes=(), inc=True):
        deps = []
        for t in reads:
            deps.append(t.w)
        for t in writes:
            deps.append(t.w)
            deps.extend(t.r)
        self._wait(ek, deps)
        ins = fn(self.eng[ek])
        if inc:
            self.cnt[ek] += 1
            ins.then_inc(self.sem[ek], 1)
            ev = Ev(self.sem[ek], self.cnt[ek], ek)
            for p in self.pending[ek]:
                p.val = self.cnt[ek]
            self.pending[ek] = []
        else:
            ev = Ev(self.sem[ek], None, ek)
            self.pending[ek].append(ev)
        self._record(ev, reads, writes)
        return ev

    def dma(self, qk, out, in_, reads=(), writes=(), st=None, **kw):
        deps = []
        for t in reads:
            deps.append(t.w)
        for t in writes:
            deps.append(t.w)
            deps.extend(t.r)
        self._wait(qk, deps)
        if st.dsem is None:
            st.dsem = {}
        if qk not in st.dsem:
            st.dsem[qk] = [self.es.enter_context(self.nc.semaphore("dsem%d" % self.nsem)), 0]
            self.dsems.append(st.dsem[qk])
            self.nsem += 1
        ds = st.dsem[qk]
        ds[1] += 16
        self.eng[qk].dma_start(out=out, in_=in_, **kw).then_inc(ds[0], 16)
        ev = Ev(ds[0], ds[1], "dma")
        self._record(ev, reads, writes)
        return ev

    def barrier(self):
        for ek in self.eng:
            assert not self.pending[ek]
        for ek in self.eng:
            w = self.waited[ek]
            for fk in self.eng:
                if fk != ek and self.cnt[fk] > 0 and w.get(id(self.sem[fk]), 0) < self.cnt[fk]:
                    self.eng[ek].wait_ge(self.sem[fk], self.cnt[fk])
                    w[id(self.sem[fk])] = self.cnt[fk]
            for ds in self.dsems:
                if ds[1] > 0 and w.get(id(ds[0]), 0) < ds[1]:
                    self.eng[ek].wait_ge(ds[0], ds[1])
                    w[id(ds[0])] = ds[1]

    def wait_all(self, ek, tiles):
        deps = []
        for t in tiles:
            deps.append(t.w)
            deps.extend(t.r)
        self._wait(ek, deps)


DBG = {}


def build_nc(debug_x1=False, phases=(1, 2)):
    nc = bass.Bass("TRN2", target_bir_lowering=False)
    x_d = nc.dram_tensor("x", [L, D], F32, kind="ExternalInput").ap()
    cv_d = nc.dram_tensor("cvec", [128, NV], F32, kind="ExternalInput").ap()
    cst_d = nc.dram_tensor("consts", [128, 256], F32, kind="ExternalInput").ap()
    w_in_d = nc.dram_tensor("w_in", [D, IN_TOTAL], F32, kind="ExternalInput").ap()
    wa_d = nc.dram_tensor("rg_wa", [16, 64, 64], F32, kind="ExternalInput").ap()
    wx_d = nc.dram_tensor("rg_wx", [16, 64, 64], F32, kind="ExternalInput").ap()
    woa_d = nc.dram_tensor("w_o_attn", [512, D], F32, kind="ExternalInput").ap()
    wor_d = nc.dram_tensor("w_o_rnn", [D, D], F32, kind="ExternalInput").ap()
    wout_d = nc.dram_tensor("w_out", [D, D], F32, kind="ExternalInput").ap()
    wup_d = nc.dram_tensor("w_up", [D, 2 * DFF], F32, kind="ExternalInput").ap()
    wdn_d = nc.dram_tensor("w_down", [DFF, D], F32, kind="ExternalInput").ap()
    out_d = nc.dram_tensor("out", [L, D], F32, kind="ExternalOutput").ap()
    x1_d = None
    if 1 in phases:
        x1_d = nc.dram_tensor("x1s", [L, D], F32, kind="ExternalOutput" if debug_x1 else "Internal").ap()
        w_in_b = nc.dram_tensor("w_in_b", [D, IN_TOTAL], BF16, kind="Internal").ap()
        woa_b = nc.dram_tensor("woa_b", [512, D], BF16, kind="Internal").ap()
        wor_b = nc.dram_tensor("wor_b", [D, D], BF16, kind="Internal").ap()
        wout_b = nc.dram_tensor("wout_b", [D, D], BF16, kind="Internal").ap()

    with ExitStack() as es:
        P = Prog(nc, es)

        def sb(name, shape, dt):
            return es.enter_context(nc.sbuf_tensor(name, shape, dt))

        def ps(name, shape, dt):
            return es.enter_context(nc.psum_tensor(name, shape, dt))

        cv = sb("cv", [128, NV], F32)
        cv_t = Tile("cv")
        P.dma("sp", cv[:], cv_d, writes=[cv_t], st=cv_t)
        cst = sb("cst", [128, 256], F32)
        cst_t = Tile("cst")
        P.dma("sp", cst[:], cst_d, writes=[cst_t], st=cst_t)
        identb = sb("identb", [128, 128], BF16)
        ident_t = Tile("ident")
        P.op("dve", lambda e: e.tensor_copy(out=identb[:], in_=cst[:, 0:128]), reads=[cst_t], writes=[ident_t])

        pbanks = [ps("pb%d" % i, [128, 512], F32) for i in range(6)]
        pb_tiles = [Tile("pb%d" % i) for i in range(6)]
        ptr = [ps("ptr%d" % i, [128, 1024], BF16) for i in range(2)]
        ptr_t = [Tile("ptr0"), Tile("ptr1")]

        x1_t = [Tile("x1_%d" % i) for i in range(L // 512)]
        if 1 in phases:
            phase1(nc, P, x_d, x1_d, x1_t, w_in_d, wa_d, wx_d, woa_d, wor_d, wout_d, (w_in_b, woa_b, wor_b, wout_b),
                   cv, cv_t, cst, cst_t, identb, ident_t, pbanks, pb_tiles, ptr, ptr_t)
        if 1 in phases and 2 in phases:
            P.barrier()
        if 2 in phases:
            phase2(nc, P, es, sb, x1_t if 1 in phases else None, x1_d if 1 in phases else x_d, out_d, wup_d, wdn_d, cv, cv_t, identb, ident_t,
                   pbanks, pb_tiles, ptr, ptr_t)
    return nc


def phase1(nc, P, x_d, x1_d, x1_t, w_in_d, wa_d, wx_d, woa_d, wor_d, wout_d, scr, cv, cv_t, cst, cst_t,
           identb, ident_t, pbanks, pb_tiles, ptr, ptr_t):
    T = 512
    NS = DBG.get("ns1", L // T)
    NIT = DBG.get("nit", 13)
    w_in_b, woa_b, wor_b, wout_b = scr
    with ExitStack() as es:
        def sb(name, shape, dt):
            return es.enter_context(nc.sbuf_tensor(name, shape, dt))

        NSLOT = 4
        slots = [sb("wslot%d" % i, [128, 8, 512], BF16) for i in range(NSLOT)]
        slot_pool = Pool([(slots[i], Tile("wslot%d" % i)) for i in range(NSLOT)])
        win_v = w_in_d.rearrange("(c p) n -> p c n", p=128)
        winb_v = w_in_b.rearrange("(c p) n -> p c n", p=128)
        woa_v = woa_d.rearrange("(h p) n -> p h n", p=64)
        woab_v = woa_b.rearrange("(h p) n -> p h n", p=64)
        wor_v = wor_d.rearrange("(c p) n -> p c n", p=128)
        worb_v = wor_b.rearrange("(c p) n -> p c n", p=128)
        wout_v = wout_d.rearrange("(c p) n -> p c n", p=128)
        woutb_v = wout_b.rearrange("(c p) n -> p c n", p=128)
        scr_t = {}

        def convert(name, src_v, dst_v, npart, ncols):
            c0 = 0
            while c0 < ncols:
                w = min(512, ncols - c0)
                sl, sl_t = slot_pool.get()
                P.dma("pool", sl[0:npart, :, 0:w], src_v[:, :, c0:c0 + w], writes=[sl_t], st=sl_t)
                t = Tile("scr_%s_%d" % (name, c0))
                scr_t[(name, c0)] = t
                P.dma("sp", dst_v[:, :, c0:c0 + w], sl[0:npart, :, 0:w], reads=[sl_t], writes=[t], st=sl_t)
                c0 += w

        def do_convert():
            convert("win", win_v, winb_v, 128, IN_TOTAL)
            convert("woa", woa_v, woab_v, 64, D)
            convert("wor", wor_v, worb_v, 128, D)
            convert("wout", wout_v, woutb_v, 128, D)

        def scr_tiles(name, c0, w):
            out = []
            for (n, cc), t in scr_t.items():
                if n == name and cc < c0 + w and c0 < cc + 512:
                    out.append(t)
            return out

        def wload(sl, sl_t, name, view, npart, c0, w, off=0):
            P.dma("sp", sl[0:npart, :, off:off + w], view[:, :, c0:c0 + w], reads=scr_tiles(name, c0, w),
                  writes=[sl_t], st=sl_t)

        KT = sb("KT", [128, 2, L], BF16)
        kt_t = [Tile("kt%d" % i) for i in range(L // T)]
        VA = sb("VA", [128, 32, 2, 128], BF16)
        va_t = [Tile("va%d" % i) for i in range(32)]
        kiT = sb("kiT", [128, L], BF16)
        kit_t = [Tile("kit%d" % i) for i in range(L // T)]
        rxhist = sb("rxhist", [128, 8, 3], F32)
        rxh_t = [Tile("rxh%d" % i) for i in range(8)]
        hst = sb("hst", [128, 8], F32)
        hst_t = [Tile("hst%d" % i) for i in range(8)]
        BDa = sb("BDa", [128, 8, 128], BF16)
        BDx = sb("BDx", [128, 8, 128], BF16)
        bd_t = Tile("bd")
        clam = sb("clam", [128, 16], F32)
        clam_t = Tile("clam")
        I4 = sb("I4", [128, 4, 128], BF16)
        i4_t = Tile("I4")
        ones64 = sb("ones64", [64, 64], BF16)
        ones_t = Tile("ones64")

        P.op("pool", lambda e: e.memset(VA[:, :, :, 64:128], 1.0), writes=va_t)
        P.op("pool", lambda e: e.memset(KT[64:128, :, :], 0.0), writes=kt_t)
        P.op("pool", lambda e: e.memset(kiT[64:128, :], 0.0), writes=kit_t)
        P.op("pool", lambda e: e.memset(rxhist[:], 0.0), writes=rxh_t)
        P.op("pool", lambda e: e.memset(hst[:], 0.0), writes=hst_t)
        P.op("pool", lambda e: e.memset(BDa[:], 0.0), writes=[bd_t])
        P.op("pool", lambda e: e.memset(BDx[:], 0.0), writes=[bd_t])
        P.op("pool", lambda e: e.memset(ones64[:], 1.0 / 64.0), writes=[ones_t])
        for (bd, src) in ((BDa, wa_d), (BDx, wx_d)):
            v = src.rearrange("(m two) d e -> two d m e", two=2)
            P.dma("pool", bd[0:64, :, 0:64], v[0], writes=[bd_t], st=bd_t)
            P.dma("pool", bd[64:128, :, 64:128], v[1], writes=[bd_t], st=bd_t)
        for r in range(4):
            P.op("dve", lambda e, r=r: e.tensor_copy(out=I4[:, r, :], in_=cst[:, 0:128]), reads=[cst_t], writes=[i4_t])
        lam = CV["lam"]
        P.op("act", lambda e: e.activation(out=clam[:, 0:8], in_=cv[:, lam:lam + 8], func=AF.Exp, scale=-1.0),
             reads=[cv_t], writes=[clam_t])
        P.op("act", lambda e: e.activation(out=clam[:, 0:8], in_=clam[:, 0:8], func=AF.Ln, bias=1.0),
             reads=[clam_t], writes=[clam_t])
        P.op("dve", lambda e: e.tensor_scalar(out=clam[:, 8:16], in0=clam[:, 0:8], scalar1=-16.0, scalar2=None, op0=ALU.mult),
             reads=[clam_t], writes=[clam_t])
        P.op("dve", lambda e: e.tensor_scalar(out=clam[:, 0:8], in0=clam[:, 0:8], scalar1=-8.0, scalar2=None, op0=ALU.mult),
             reads=[clam_t], writes=[clam_t])

        big = sb("big", [128, 4096], F32)
        big_t = Tile("big")
        xb = sb("xb", [128, 4, D], F32)
        xb_t = Tile("xb")
        xs = sb("xs1", [128, 4, D], BF16)
        xs_t = Tile("xs1")
        mrgT = xs[:].rearrange("p a (c t) -> p (a c) t", t=T)
        mrg_t = xs_t
        stat = sb("stat1", [128, 8], F32)
        stat_t = Tile("stat1")
        xnTs = [sb("xnT%d" % i, [128, 8, T], BF16) for i in range(2)]
        xnT_ts = [Tile("xnT%d" % i) for i in range(2)]
        qT = sb("qT", [128, 8, T], BF16)
        qT_t = Tile("qT")
        qiT = sb("qiT", [128, 4, T], BF16)
        qiT_t = Tile("qiT")
        P.op("pool", lambda e: e.memset(qT[64:128, :, :], 0.0), writes=[qT_t])
        P.op("pool", lambda e: e.memset(qiT[64:128, :, :], 0.0), writes=[qiT_t])
        wabs = sb("wabs", [128, 4, 4], F32)
        wsgn = sb("wsgn", [128, 4, 4], F32)
        wi_t = [Tile("wi%d" % a) for a in range(4)]
        attnT = sb("attnT", [64, 8, T], BF16)
        attn_t = Tile("attnT")
        rnnT = sb("rnnT", [128, 8, T], BF16)
        rnn_t = Tile("rnnT")
        MBs = [sb("MB%d" % i, [128, 4096], BF16) for i in range(2)]
        mb_ts = [(Tile("MBd%d" % i), Tile("MBa%d" % i)) for i in range(2)]
        bis = sb("bis", [128, 8], F32)
        bis_t = Tile("bis")
        cand = sb("cand", [128, 2], F32)
        cand_t = Tile("cand")
        MS = sb("MS", [128, 16], F32)
        ms_t = Tile("MS")
        stpc = sb("stpc", [128, 16], F32)
        stpc_t = Tile("stpc")
        for k in range(1, 17):
            P.op("pool", lambda e, k=k: e.memset(stpc[:, k - 1:k], 2.0 ** (1 - k)), writes=[stpc_t])
        NF = 8
        ftmp = [sb("ft%d" % i, [128, 3 + T], F32) for i in range(NF)]
        f_pool = Pool([(ftmp[i], Tile("ft%d" % i)) for i in range(NF)])
        NB = 5
        btmp = [sb("bt%d" % i, [128, T], BF16) for i in range(NB)]
        b_pool = Pool([(btmp[i], Tile("bt%d" % i)) for i in range(NB)])
        pb_pool = Pool(list(zip(pbanks[0:4], pb_tiles[0:4])))
        OT = [(pbanks[4], pb_tiles[4]), (pbanks[5], pb_tiles[5])]
        ptr_pool = Pool([(ptr[0], ptr_t[0]), (ptr[1], ptr_t[1])])
        g1 = CV["g1"]
        cw = CV["convw"]

        def proj(pb, pb_t, M, sl, sl_t, c0, xnT, xnT_t):
            for c in range(8):
                P.op("pe", lambda e, c=c: e.matmul(pb[0:M, 0:T], lhsT=sl[:, c, c0:c0 + M], rhs=xnT[:, c, :],
                                                 start=(c == 0), stop=(c == 7)),
                     reads=[xnT_t, sl_t], writes=[pb_t], inc=(c == 7))

        def qknorm(pb, pb_t, out_ap, out_tiles, gcol):
            qf, qf_t = f_pool.get()
            sq, sq_t = b_pool.get()
            P.op("act", lambda e: e.activation(out=qf[0:64, 0:T], in_=pb[0:64, 0:T], func=AF.Copy, scale=cv[0:64, gcol:gcol + 1]),
                 reads=[pb_t, cv_t], writes=[qf_t])
            P.op("act", lambda e: e.activation(out=sq[0:64, :], in_=pb[0:64, 0:T], func=AF.Square), reads=[pb_t], writes=[sq_t])
            p2, p2_t = pb_pool.get()
            P.op("pe", lambda e: e.matmul(p2[0:64, 0:T], lhsT=ones64[:], rhs=sq[0:64, :], start=True, stop=True),
                 reads=[sq_t, ones_t], writes=[p2_t])
            sd, sd_t = f_pool.get()
            P.op("act", lambda e: e.activation(out=sd[0:64, 0:T], in_=p2[0:64, 0:T], func=AF.Ln, bias=EPS), reads=[p2_t], writes=[sd_t])
            P.op("act", lambda e: e.activation(out=sd[0:64, 0:T], in_=sd[0:64, 0:T], func=AF.Exp, scale=-0.5), reads=[sd_t], writes=[sd_t])
            P.op("pool", lambda e: e.tensor_tensor(out=out_ap, in0=qf[0:64, 0:T], in1=sd[0:64, 0:T], op=ALU.mult),
                 reads=[qf_t, sd_t], writes=out_tiles)

        def x_view(s):
            return x_d[s * T:(s + 1) * T, :].rearrange("(a p) d -> p a d", p=128)

        def stage_A(s):
            xnT, xnT_t = xnTs[s % 2], xnT_ts[s % 2]
            P.dma("sp", xb[:], x_view(s), writes=[xb_t], st=xb_t)
            for a in range(4):
                P.op("act", lambda e, a=a: e.activation(out=xs[:, a, :], in_=xb[:, a, :], func=AF.Square,
                                                        accum_out=stat[:, a:a + 1]),
                     reads=[xb_t], writes=[xs_t, stat_t])
            P.op("act", lambda e: e.activation(out=stat[:, 0:4], in_=stat[:, 0:4], func=AF.Ln, scale=1.0 / D, bias=EPS),
                 reads=[stat_t], writes=[stat_t])
            P.op("act", lambda e: e.activation(out=stat[:, 4:8], in_=stat[:, 0:4], func=AF.Exp, scale=-0.5),
                 reads=[stat_t], writes=[stat_t])
            for a in range(4):
                P.op("act", lambda e, a=a: e.activation(out=xs[:, a, :], in_=xb[:, a, :], func=AF.Copy,
                                                        scale=stat[:, 4 + a:5 + a]),
                     reads=[xb_t, stat_t], writes=[xs_t])
            for c in range(8):
                tp, tp_t = ptr_pool.get()
                for a in range(4):
                    P.op("pe", lambda e, a=a, c=c, tp=tp: e.transpose(out=tp[:, a * 128:(a + 1) * 128],
                                                                      in_=xs[:, a, c * 128:(c + 1) * 128],
                                                                      identity=identb[:]),
                         reads=[xs_t, ident_t], writes=[tp_t], inc=(a == 3))
                P.op("act", lambda e, c=c, tp=tp: e.activation(out=xnT[:, c, :], in_=tp[:, 0:T], func=AF.Copy,
                                                               scale=cv[:, g1 + c:g1 + c + 1]),
                     reads=[tp_t, cv_t], writes=[xnT_t])

        def stage_B_idx(s, slk, slk_t, slw, slw_t):
            xnT, xnT_t = xnTs[s % 2], xnT_ts[s % 2]
            tok0 = s * T
            for g in range(2):
                pb, pb_t = pb_pool.get()
                proj(pb, pb_t, 64, slk, slk_t, g * 64, xnT, xnT_t)
                qknorm(pb, pb_t, KT[0:64, g, tok0:tok0 + T], [kt_t[s]], CV["kg"])
            for h in range(4):
                pb, pb_t = pb_pool.get()
                proj(pb, pb_t, 64, slk, slk_t, 256 + h * 64, xnT, xnT_t)
                P.op("act", lambda e, h=h, pb=pb: e.activation(out=qiT[0:64, h, :], in_=pb[0:64, 0:T], func=AF.Copy),
                     reads=[pb_t], writes=[qiT_t])
            pb, pb_t = pb_pool.get()
            proj(pb, pb_t, 64, slw, slw_t, 0, xnT, xnT_t)
            qknorm(pb, pb_t, kiT[0:64, tok0:tok0 + T], [kit_t[s]], CV["kig"])
            for a in range(4):
                j = 4 * s + a
                pb, pb_t = pb_pool.get()
                for c in range(8):
                    P.op("pe", lambda e, c=c, a=a, pb=pb: e.matmul(pb[:, 0:128], lhsT=xnT[:, c, a * 128:(a + 1) * 128],
                                                                 rhs=slk[:, c, 128:256], start=(c == 0), stop=(c == 7)),
                         reads=[xnT_t, slk_t], writes=[pb_t], inc=(c == 7))
                P.op("act", lambda e, j=j, pb=pb: e.activation(out=VA[:, j, :, 0:64],
                                                               in_=pb[:, 0:128].rearrange("p (g d) -> p g d", g=2),
                                                               func=AF.Copy),
                     reads=[pb_t], writes=[va_t[j]])
                pb, pb_t = pb_pool.get()
                for c in range(8):
                    P.op("pe", lambda e, c=c, a=a, pb=pb: e.matmul(pb[:, 0:4], lhsT=xnT[:, c, a * 128:(a + 1) * 128],
                                                                 rhs=slw[:, c, 64:68], start=(c == 0), stop=(c == 7)),
                         reads=[xnT_t, slw_t], writes=[pb_t], inc=(c == 7))
                P.op("act", lambda e, a=a, pb=pb: e.activation(out=wabs[:, a, :], in_=pb[:, 0:4], func=AF.Abs, scale=1.0 / 16.0),
                     reads=[pb_t], writes=[wi_t[a]])
                P.op("act", lambda e, a=a, pb=pb: e.activation(out=wsgn[:, a, :], in_=pb[:, 0:4], func=AF.Sign),
                     reads=[pb_t], writes=[wi_t[a]])

        def stage_B_q(s, slq, slq_t):
            xnT, xnT_t = xnTs[s % 2], xnT_ts[s % 2]
            for h in range(8):
                pb, pb_t = pb_pool.get()
                proj(pb, pb_t, 64, slq, slq_t, h * 64, xnT, xnT_t)
                qknorm(pb, pb_t, qT[0:64, h, :], [qT_t], CV["qg"])

        def att_scores(s, a):
            i = 4 * s + a
            n = 128 * (i + 1)
            qs = slice(a * 128, (a + 1) * 128)
            nch = (n + 511) // 512
            for c in range(nch):
                w = min(512, n - 512 * c)
                ks = slice(512 * c, 512 * c + w)
                for h in range(4):
                    pb, pb_t = pb_pool.get()
                    P.op("pe", lambda e, h=h, pb=pb, w=w, ks=ks: e.matmul(pb[:, 0:w], lhsT=qiT[:, h, qs], rhs=kiT[:, ks],
                                                                         start=True, stop=True),
                         reads=[qiT_t] + kit_t[0:s + 1], writes=[pb_t])
                    r, r_t = f_pool.get()
                    P.op("act", lambda e, h=h, pb=pb, r=r, w=w: e.activation(out=r[:, 0:w], in_=pb[:, 0:w], func=AF.Relu,
                                                                           scale=wabs[:, a, h:h + 1]),
                         reads=[pb_t, wi_t[a]], writes=[r_t])
                    if h == 0:
                        P.op("dve", lambda e, r=r, w=w, ks=ks: e.tensor_scalar(out=big[:, ks], in0=r[:, 0:w],
                                                                              scalar1=wsgn[:, a, 0:1], scalar2=None,
                                                                              op0=ALU.mult),
                             reads=[r_t, wi_t[a]], writes=[big_t])
                    else:
                        P.op("dve", lambda e, h=h, r=r, w=w, ks=ks: e.scalar_tensor_tensor(
                            out=big[:, ks], in0=r[:, 0:w], scalar=wsgn[:, a, h:h + 1], in1=big[:, ks],
                            op0=ALU.mult, op1=ALU.add),
                            reads=[r_t, wi_t[a]], writes=[big_t])
            if i >= 2:
                P.op("dve", lambda e: e.tensor_reduce(out=bis[:, 0:1], in_=big[:, 0:n], axis=AX.X, op=ALU.max,
                                                      apply_absolute_value=True),
                     reads=[big_t], writes=[bis_t])
                P.op("dve", lambda e: e.tensor_scalar(out=bis[:, 1:2], in0=bis[:, 0:1], scalar1=-1.0, scalar2=None, op0=ALU.mult),
                     reads=[bis_t], writes=[bis_t])
            else:
                P.op("dve", lambda e: e.memset(bis[:, 1:2], -1e29), writes=[bis_t])
            P.op("dve", lambda e: e.tensor_tensor(out=big[:, n - 128:n], in0=big[:, n - 128:n], in1=cst[:, 128:256], op=ALU.add),
                 reads=[cst_t, big_t], writes=[big_t])

        def att_bisect(s, a):
            i = 4 * s + a
            n = 128 * (i + 1)
            MB = MBs[i % 2]
            mbD_t, mbA_t = mb_ts[i % 2]
            nit = NIT - (2 if n <= 1024 else 1 if n <= 2048 else 0)
            if i >= 2:
                P.op("dve", lambda e: e.tensor_scalar(out=MS[:, 0:nit], in0=stpc[:, 0:nit], scalar1=bis[:, 0:1], scalar2=None, op0=ALU.mult),
                     reads=[bis_t, stpc_t], writes=[ms_t])
                P.op("dve", lambda e: e.memset(cand[:, 0:1], 0.0), writes=[cand_t])
                for k in range(1, nit + 1):
                    P.op("dve", lambda e: e.tensor_scalar(out=MB[:, 0:n], in0=big[:, 0:n], scalar1=cand[:, 0:1], scalar2=None,
                                                          op0=ALU.is_ge, op1=ALU.add, accum_out=bis[:, 3:4]),
                         reads=[big_t, cand_t], writes=[mbD_t, bis_t])
                    P.op("dve", lambda e: e.tensor_scalar(out=bis[:, 4:5], in0=bis[:, 3:4], scalar1=float(TOPK) - 0.5, scalar2=-0.5,
                                                          op0=ALU.is_ge, op1=ALU.add),
                         reads=[bis_t], writes=[bis_t])
                    P.op("dve", lambda e, k=k: e.scalar_tensor_tensor(out=cand[:, 0:1], in0=bis[:, 4:5], scalar=MS[:, k - 1:k], in1=cand[:, 0:1],
                                                                      op0=ALU.mult, op1=ALU.add),
                         reads=[bis_t, ms_t, cand_t], writes=[cand_t])
                P.op("dve", lambda e: e.scalar_tensor_tensor(out=bis[:, 1:2], in0=MS[:, nit - 1:nit], scalar=-0.5, in1=cand[:, 0:1],
                                                             op0=ALU.mult, op1=ALU.add),
                     reads=[ms_t, cand_t], writes=[bis_t])
            P.op("dve", lambda e: e.tensor_scalar(out=MB[:, 0:n], in0=big[:, 0:n], scalar1=bis[:, 1:2], scalar2=NEG,
                                                  op0=ALU.is_lt, op1=ALU.mult),
                 reads=[big_t, bis_t], writes=[mbD_t, mbA_t])

        def att_main(s, a):
            i = 4 * s + a
            qs = slice(a * 128, (a + 1) * 128)
            MB = MBs[i % 2]
            mb_tt = list(mb_ts[i % 2])
            items = [(j, g) for j in range(i + 1) for g in range(2)]

            def st_exp(j, g):
                kb = slice(128 * j, 128 * j + 128)
                pb, pb_t = pb_pool.get()
                P.op("pe", lambda e: e.matmul(pb[:, :], lhsT=KT[:, g, kb], rhs=qT[:, 4 * g:4 * g + 4, qs],
                                              start=True, stop=False),
                     reads=[qT_t] + kt_t[0:s + 1], writes=[pb_t], inc=False)
                P.op("pe", lambda e: e.matmul(pb[:, :], lhsT=MB[:, kb], rhs=I4[:].rearrange("p r q -> p (r q)"),
                                              start=False, stop=True),
                     reads=mb_tt + [i4_t], writes=[pb_t])
                pt, pt_t = b_pool.get()
                P.op("act", lambda e: e.activation(out=pt[:], in_=pb[:, :], func=AF.Exp, scale=0.125),
                     reads=[pb_t], writes=[pt_t])
                return pt, pt_t

            def pv(j, g, pt, pt_t):
                ot, ot_t = OT[g]
                P.op("pe", lambda e: e.matmul(ot[:, :], lhsT=VA[:, j, g, :], rhs=pt[:],
                                              start=(j == 0), stop=(j == i)),
                     reads=[pt_t, va_t[j]], writes=[ot_t], inc=(j == i))

            LOOK = 2
            pend = []
            for k, (j, g) in enumerate(items):
                pend.append((j, g) + st_exp(j, g))
                if len(pend) > LOOK:
                    pv(*pend.pop(0))
            while pend:
                pv(*pend.pop(0))
            for g in range(2):
                ot, ot_t = OT[g]
                rc, rc_t = f_pool.get()
                P.op("act", lambda e, ot=ot, rc=rc: e.activation(out=rc[0:64, 0:T], in_=ot[64:128, :], func=AF.Ln), reads=[ot_t], writes=[rc_t])
                P.op("act", lambda e, rc=rc: e.activation(out=rc[0:64, 0:T], in_=rc[0:64, 0:T], func=AF.Exp, scale=-1.0), reads=[rc_t], writes=[rc_t])
                P.op("dve", lambda e, g=g, ot=ot, rc=rc: e.tensor_tensor(
                    out=attnT[:, 4 * g:4 * g + 4, qs], in0=ot[0:64, :].rearrange("p (r q) -> p r q", r=4),
                    in1=rc[0:64, 0:T].rearrange("p (r q) -> p r q", r=4), op=ALU.mult),
                    reads=[ot_t, rc_t], writes=[attn_t])

        rnn_state = {}
        xc_ded = [(sb("xcd%d" % i, [128, T], F32), Tile("xcd%d" % i)) for i in range(2)]
        xcb_ded = [(sb("xcbd%d" % i, [128, T], BF16), Tile("xcbd%d" % i)) for i in range(2)]

        def rnn_part1(s, m, slr, slr_t):
            xnT, xnT_t = xnTs[s % 2], xnT_ts[s % 2]
            mm = m % 4
            pb, pb_t = pb_pool.get()
            proj(pb, pb_t, 128, slr, slr_t, mm * 128, xnT, xnT_t)
            rxh, rxh_tt = f_pool.get()
            P.op("pool", lambda e: e.tensor_copy(out=rxh[:, 0:3], in_=rxhist[:, m, :]), reads=[rxh_t[m]], writes=[rxh_tt])
            P.op("act", lambda e: e.activation(out=rxh[:, 3:3 + T], in_=pb[:, :], func=AF.Copy), reads=[pb_t], writes=[rxh_tt])
            P.op("pool", lambda e: e.tensor_copy(out=rxhist[:, m, :], in_=rxh[:, T:T + 3]), reads=[rxh_tt], writes=[rxh_t[m]])
            xc, xc_t = xc_ded[m % 2]
            P.op("dve", lambda e: e.tensor_scalar(
                out=xc[:, 0:T], in0=rxh[:, 3:3 + T], scalar1=cv[:, cw + 3 * 8 + m:cw + 3 * 8 + m + 1],
                scalar2=cv[:, CV["convb"] + m:CV["convb"] + m + 1], op0=ALU.mult, op1=ALU.add),
                reads=[rxh_tt, cv_t], writes=[xc_t])
            for jj in (2, 1, 0):
                P.op("dve", lambda e, jj=jj: e.scalar_tensor_tensor(
                    out=xc[:, 0:T], in0=rxh[:, jj:jj + T], scalar=cv[:, cw + jj * 8 + m:cw + jj * 8 + m + 1],
                    in1=xc[:, 0:T], op0=ALU.mult, op1=ALU.add),
                    reads=[rxh_tt, cv_t], writes=[xc_t])
            xcb, xcb_t = xcb_ded[m % 2]
            P.op("act", lambda e: e.activation(out=xcb[:], in_=xc[:, 0:T], func=AF.Copy), reads=[xc_t], writes=[xcb_t])
            rnn_state[m] = (xc, xc_t, xcb, xcb_t)

        def rnn_part2(s, m, slg, slg_t):
            xnT, xnT_t = xnTs[s % 2], xnT_ts[s % 2]
            mm = m % 4
            xc, xc_t, xcb, xcb_t = rnn_state.pop(m)
            pr, pr_t = pb_pool.get()
            P.op("pe", lambda e: e.matmul(pr[:, :], lhsT=BDa[:, m, :], rhs=xcb[:], start=True, stop=True),
                 reads=[xcb_t, bd_t], writes=[pr_t])
            pi, pi_t = pb_pool.get()
            P.op("pe", lambda e: e.matmul(pi[:, :], lhsT=BDx[:, m, :], rhs=xcb[:], start=True, stop=True),
                 reads=[xcb_t, bd_t], writes=[pi_t])
            rr, rr_t = f_pool.get()
            P.op("act", lambda e: e.activation(out=rr[:, 0:T], in_=pr[:, :], func=AF.Sigmoid,
                                               bias=cv[:, CV["ba"] + m:CV["ba"] + m + 1]),
                 reads=[pr_t, cv_t], writes=[rr_t])
            ii, ii_t = f_pool.get()
            P.op("act", lambda e: e.activation(out=ii[:, 0:T], in_=pi[:, :], func=AF.Sigmoid,
                                               bias=cv[:, CV["bx"] + m:CV["bx"] + m + 1]),
                 reads=[pi_t, cv_t], writes=[ii_t])
            aa, aa_t = f_pool.get()
            P.op("act", lambda e: e.activation(out=aa[:, 0:T], in_=rr[:, 0:T], func=AF.Exp, scale=clam[:, m:m + 1]),
                 reads=[rr_t, clam_t], writes=[aa_t])
            P.op("act", lambda e: e.activation(out=rr[:, 0:T], in_=rr[:, 0:T], func=AF.Exp, scale=clam[:, 8 + m:9 + m]),
                 reads=[rr_t, clam_t], writes=[rr_t])
            P.op("act", lambda e: e.activation(out=rr[:, 0:T], in_=rr[:, 0:T], func=AF.Relu, scale=-1.0, bias=1.0),
                 reads=[rr_t], writes=[rr_t])
            P.op("act", lambda e: e.activation(out=rr[:, 0:T], in_=rr[:, 0:T], func=AF.Sqrt, bias=1e-30),
                 reads=[rr_t], writes=[rr_t])
            P.op("dve", lambda e: e.tensor_tensor(out=ii[:, 0:T], in0=ii[:, 0:T], in1=xc[:, 0:T], op=ALU.mult),
                 reads=[ii_t, xc_t], writes=[ii_t])
            P.op("dve", lambda e: e.tensor_tensor(out=ii[:, 0:T], in0=ii[:, 0:T], in1=rr[:, 0:T], op=ALU.mult),
                 reads=[ii_t, rr_t], writes=[ii_t])
            hh, hh_t = f_pool.get()
            P.op("dve", lambda e: e.tensor_tensor_scan(
                out=hh[:, 0:T], data0=aa[:, 0:T], data1=ii[:, 0:T], initial=hst[:, m:m + 1], op0=ALU.mult, op1=ALU.add),
                reads=[aa_t, ii_t, hst_t[m]], writes=[hh_t])
            P.op("pool", lambda e: e.tensor_copy(out=hst[:, m:m + 1], in_=hh[:, T - 1:T]), reads=[hh_t], writes=[hst_t[m]])
            pg, pg_t = pb_pool.get()
            proj(pg, pg_t, 128, slg, slg_t, mm * 128, xnT, xnT_t)
            gl, gl_t = f_pool.get()
            P.op("act", lambda e: e.activation(out=gl[:, 0:T], in_=pg[:, :], func=AF.Gelu_apprx_tanh), reads=[pg_t], writes=[gl_t])
            P.op("dve", lambda e: e.tensor_tensor(out=rnnT[:, m, :], in0=hh[:, 0:T], in1=gl[:, 0:T], op=ALU.mult),
                 reads=[hh_t, gl_t], writes=[rnn_t])

        def merge_pair(s, fp, sg_, sg_t, so_, so_t):
            xnT, xnT_t = xnTs[s % 2], xnT_ts[s % 2]
            for ff in range(2):
                f = 2 * fp + ff
                pga, pga_t = pb_pool.get()
                proj(pga, pga_t, 128, sg_, sg_t, ff * 128, xnT, xnT_t)
                sa, sa_t = f_pool.get()
                P.op("act", lambda e, pga=pga, sa=sa: e.activation(out=sa[:, 0:T], in_=pga[:, :], func=AF.Sigmoid),
                     reads=[pga_t], writes=[sa_t])
                pgb, pgb_t = pb_pool.get()
                proj(pgb, pgb_t, 128, sg_, sg_t, 256 + ff * 128, xnT, xnT_t)
                sbb, sbb_t = f_pool.get()
                P.op("act", lambda e, pgb=pgb, sbb=sbb: e.activation(out=sbb[:, 0:T], in_=pgb[:, :], func=AF.Sigmoid),
                     reads=[pgb_t], writes=[sbb_t])
                pa, pa_t = pb_pool.get()
                for h in range(8):
                    P.op("pe", lambda e, h=h, ff=ff, pa=pa: e.matmul(pa[:, :], lhsT=so_[0:64, h, ff * 128:(ff + 1) * 128],
                                                                    rhs=attnT[:, h, :], start=(h == 0), stop=(h == 7)),
                         reads=[attn_t, so_t], writes=[pa_t], inc=(h == 7))
                P.op("dve", lambda e, sa=sa, pa=pa: e.tensor_tensor(out=sa[:, 0:T], in0=sa[:, 0:T], in1=pa[:, :], op=ALU.mult),
                     reads=[sa_t, pa_t], writes=[sa_t])
                pbm, pbm_t = pb_pool.get()
                for c in range(8):
                    P.op("pe", lambda e, c=c, ff=ff, pbm=pbm: e.matmul(pbm[:, :], lhsT=so_[:, c, 256 + ff * 128:256 + (ff + 1) * 128],
                                                                      rhs=rnnT[:, c, :], start=(c == 0), stop=(c == 7)),
                         reads=[rnn_t, so_t], writes=[pbm_t], inc=(c == 7))
                P.op("dve", lambda e, sbb=sbb, pbm=pbm: e.tensor_tensor(out=sbb[:, 0:T], in0=sbb[:, 0:T], in1=pbm[:, :], op=ALU.mult),
                     reads=[sbb_t, pbm_t], writes=[sbb_t])
                P.op("dve", lambda e, f=f, sa=sa, sbb=sbb: e.tensor_tensor(out=mrgT[:, f, :], in0=sa[:, 0:T], in1=sbb[:, 0:T], op=ALU.add),
                     reads=[sa_t, sbb_t], writes=[mrg_t])

        def outproj_half(s, nn, swo, swo_t):
            for a in range(4):
                pb, pb_t = pb_pool.get()
                for f in range(8):
                    P.op("pe", lambda e, a=a, f=f, pb=pb: e.matmul(pb[:, :], lhsT=mrgT[:, f, a * 128:(a + 1) * 128],
                                                                  rhs=swo[:, f, :], start=(f == 0), stop=(f == 7)),
                         reads=[mrg_t, swo_t], writes=[pb_t], inc=(f == 7))
                P.op("dve", lambda e, a=a, pb=pb: e.tensor_tensor(out=xb[:, a, nn * 512:(nn + 1) * 512], in0=pb[:, :],
                                                                 in1=xb[:, a, nn * 512:(nn + 1) * 512], op=ALU.add),
                     reads=[pb_t, xb_t], writes=[xb_t])

        slot_free = list(slot_pool.items)

        class _SP:
            @staticmethod
            def get():
                assert slot_free, "no free weight slot"
                return slot_free.pop(0)

        def release(w):
            for k in range(0, len(w), 2):
                slot_free.append((w[k], w[k + 1]))

        def L_rnn(half):
            def f():
                a_ = _SP.get(); wload(a_[0], a_[1], "win", winb_v, 128, 1092 + 512 * half, 512)
                b_ = _SP.get(); wload(b_[0], b_[1], "win", winb_v, 128, 2116 + 512 * half, 512)
                return a_ + b_
            return f

        def L_idx():
            a_ = _SP.get(); wload(a_[0], a_[1], "win", winb_v, 128, 512, 512)
            b_ = _SP.get(); wload(b_[0], b_[1], "win", winb_v, 128, 1024, 68)
            return a_ + b_

        def L_q():
            a_ = _SP.get(); wload(a_[0], a_[1], "win", winb_v, 128, 0, 512)
            return a_

        def L_merge(fp):
            def f():
                a_ = _SP.get()
                wload(a_[0], a_[1], "win", winb_v, 128, 3140 + 256 * fp, 256, off=0)
                wload(a_[0], a_[1], "win", winb_v, 128, 4164 + 256 * fp, 256, off=256)
                b_ = _SP.get()
                wload(b_[0], b_[1], "woa", woab_v, 64, 256 * fp, 256, off=0)
                wload(b_[0], b_[1], "wor", worb_v, 128, 256 * fp, 256, off=256)
                return a_ + b_
            return f

        def L_out(nn):
            def f():
                a_ = _SP.get(); wload(a_[0], a_[1], "wout", woutb_v, 128, 512 * nn, 512)
                return a_
            return f

        stage_A(0)
        do_convert()
        w_idx = L_idx()
        w_q = L_q()
        stage_B_idx(0, *w_idx)
        release(w_idx)
        stage_B_q(0, *w_q)
        release(w_q)
        w_r0 = L_rnn(0)()
        att_scores(0, 0)
        att_bisect(0, 0)
        for s in range(NS):
            last = (s == NS - 1)
            w_r1 = None
            for a in range(4):
                if a == 0:
                    w_r1 = L_rnn(1)()
                if a == 2 and not last:
                    w_idx = L_idx()
                if a == 3 and not last:
                    w_q = L_q()
                wr = w_r0 if a < 2 else w_r1
                for m in (2 * a, 2 * a + 1):
                    rnn_part1(s, m, wr[0], wr[1])
                if a < 3:
                    att_scores(s, a + 1)
                    att_bisect(s, a + 1)
                if a == 2 and not last:
                    stage_A(s + 1)
                    stage_B_idx(s + 1, *w_idx)
                    release(w_idx)
                if a == 3 and not last:
                    att_scores(s + 1, 0)
                    att_bisect(s + 1, 0)
                att_main(s, a)
                for m in (2 * a, 2 * a + 1):
                    rnn_part2(s, m, wr[2], wr[3])
                if a == 1:
                    release(w_r0)
                if a == 3:
                    release(w_r1)
            w_m0 = L_merge(0)()
            if not last:
                stage_B_q(s + 1, *w_q)
                release(w_q)
            w_m1 = L_merge(1)()
            merge_pair(s, 0, *w_m0)
            release(w_m0)
            w_m2 = L_merge(2)()
            merge_pair(s, 1, *w_m1)
            release(w_m1)
            w_m3 = L_merge(3)()
            merge_pair(s, 2, *w_m2)
            release(w_m2)
            w_o0 = L_out(0)()
            merge_pair(s, 3, *w_m3)
            release(w_m3)
            w_o1 = L_out(1)()
            P.dma("sp", xb[:], x_view(s), writes=[xb_t], st=xb_t)
            outproj_half(s, 0, *w_o0)
            release(w_o0)
            if not last:
                w_r0 = L_rnn(0)()
            outproj_half(s, 1, *w_o1)
            release(w_o1)
            P.dma("sp", x1_d[s * T:(s + 1) * T, :].rearrange("(a p) d -> p a d", p=128), xb[:],
                  reads=[xb_t], writes=[x1_t[s]], st=xb_t)
        P.wait_all("sp", [xb_t] + x1_t)


def phase2(nc, P, es2, sb_outer, x1_t, x1_d, out_d, wup_d, wdn_d, cv, cv_t, identb, ident_t, pbanks, pb_tiles, ptr, ptr_t):
    T2 = 256
    NS2 = L // T2
    with ExitStack() as es:
        def sb(name, shape, dt):
            return es.enter_context(nc.sbuf_tensor(name, shape, dt))

        wup = sb("wup", [128, 8, 2 * DFF], BF16)
        wdn = sb("wdn", [128, NFF, D], BF16)
        wup_t = [Tile("wup%d" % i) for i in range(11)]
        wdn_t = [Tile("wdn%d" % i) for i in range(2)]
        wup_v = wup_d.rearrange("(c p) n -> p c n", p=128)
        wdn_v = wdn_d.rearrange("(f p) n -> p f n", p=128)
        for i in range(11):
            P.dma("pool", wup[:, :, i * 512:(i + 1) * 512], wup_v[:, :, i * 512:(i + 1) * 512],
                  writes=[wup_t[i]], st=wup_t[i])
        for i in range(2):
            P.dma("pool", wdn[:, :, i * 512:(i + 1) * 512], wdn_v[:, :, i * 512:(i + 1) * 512],
                  writes=[wdn_t[i]], st=wdn_t[i])

        hist = sb("hist", [128, 2 * NFF, 2], F32)
        hist_t = [Tile("hist%d" % i) for i in range(2 * NFF)]
        hist_all = Tile("hist_all")
        P.op("pool", lambda e: e.memset(hist[:], 0.0), writes=hist_t)

        xts = [sb("xt%d" % i, [128, 2, D], F32) for i in range(3)]
        xt_pool = Pool([(xts[i], Tile("xt%d" % i)) for i in range(3)])
        xs = sb("xs", [128, 2, D], BF16)
        xs_t = Tile("xs")
        stat = sb("stat2", [128, 8], F32)
        stat_pool = Pool([(stat[:, 0:2], stat[:, 2:4], Tile("st2a")), (stat[:, 4:6], stat[:, 6:8], Tile("st2b"))])
        xnTs = [sb("xn2T%d" % i, [128, 8, T2], BF16) for i in range(2)]
        xnT_ts = [Tile("xn2T%d" % i) for i in range(2)]
        hTs = [sb("hT%d" % i, [128, NFF, T2], BF16) for i in range(2)]
        hT_ts = [Tile("hT%d" % i) for i in range(2)]
        upbs = [sb("upb%d" % i, [128, 2 + T2], F32) for i in range(6)]
        upb_pool = Pool([(upbs[i], Tile("upb%d" % i)) for i in range(6)])
        us = [sb("u%d" % i, [128, T2], F32) for i in range(6)]
        u_pool = Pool([(us[i], Tile("u%d" % i)) for i in range(6)])
        sgs = [sb("sg%d" % i, [128, T2], F32) for i in range(3)]
        sg_pool = Pool([(sgs[i], Tile("sg%d" % i)) for i in range(3)])
        pb_pool = Pool(list(zip(pbanks[0:5], pb_tiles[0:5])))
        pdn, pdn_t = pbanks[5], pb_tiles[5]
        ptr_pool = Pool([(ptr[0], ptr_t[0]), (ptr[1], ptr_t[1])])
        fcw = CV["fcw"]
        fcb = CV["fcb"]
        g2 = CV["g2"]
        NS2 = DBG.get('ns2', NS2)

        def load_x(s):
            xt, xt_t = xt_pool.get()
            P.dma("sp", xt[:], x1_d[s * T2:(s + 1) * T2, :].rearrange("(a p) d -> p a d", p=128),
                  reads=([x1_t[s // 2]] if x1_t is not None else []), writes=[xt_t], st=xt_t)
            return xt, xt_t

        def stage_A2(s, xt, xt_t):
            xnT, xnT_t = xnTs[s % 2], xnT_ts[s % 2]
            ss, rs, st_t = stat_pool.get()
            for a in range(2):
                P.op("act", lambda e, a=a: e.activation(out=xs[:, a, :], in_=xt[:, a, :], func=AF.Square,
                                                        accum_out=ss[:, a:a + 1]),
                     reads=[xt_t], writes=[xs_t, st_t])
            P.op("act", lambda e: e.activation(out=ss, in_=ss, func=AF.Ln, scale=1.0 / D, bias=EPS),
                 reads=[st_t], writes=[st_t])
            P.op("act", lambda e: e.activation(out=rs, in_=ss, func=AF.Exp, scale=-0.5), reads=[st_t], writes=[st_t])
            for a in range(2):
                P.op("act", lambda e, a=a: e.activation(out=xs[:, a, :], in_=xt[:, a, :], func=AF.Copy,
                                                        scale=rs[:, a:a + 1]),
                     reads=[xt_t, st_t], writes=[xs_t])
            for c in range(8):
                tp, tp_t = ptr_pool.get()
                for a in range(2):
                    P.op("pe", lambda e, a=a, c=c, tp=tp: e.transpose(out=tp[:, a * 128:(a + 1) * 128],
                                                                      in_=xs[:, a, c * 128:(c + 1) * 128],
                                                                      identity=identb[:]),
                         reads=[xs_t, ident_t], writes=[tp_t], inc=(a == 1))
                P.op("act", lambda e, c=c, tp=tp: e.activation(out=xnT[:, c, :], in_=tp[:, 0:T2], func=AF.Copy,
                                                               scale=cv[:, g2 + c:g2 + c + 1]),
                     reads=[tp_t, cv_t], writes=[xnT_t])

        def down_ops(s, xt, xt_t):
            hT, hT_t = hTs[s % 2], hT_ts[s % 2]
            for a in range(2):
                for n in range(2):
                    for f in range(NFF):
                        P.op("pe", lambda e, a=a, n=n, f=f: e.matmul(pdn[:, :], lhsT=hT[:, f, a * 128:(a + 1) * 128],
                                                                   rhs=wdn[:, f, n * 512:(n + 1) * 512],
                                                                   start=(f == 0), stop=(f == NFF - 1)),
                             reads=[hT_t, wdn_t[n]], writes=[pdn_t], inc=(f == NFF - 1))
                        if f == NFF - 1:
                            P.op("dve", lambda e, a=a, n=n: e.tensor_tensor(out=xt[:, a, n * 512:(n + 1) * 512], in0=pdn[:, :],
                                                                           in1=xt[:, a, n * 512:(n + 1) * 512], op=ALU.add),
                                 reads=[pdn_t, xt_t], writes=[xt_t])
                        yield
            P.dma("sp", out_d[s * T2:(s + 1) * T2, :].rearrange("(a p) d -> p a d", p=128), xt[:],
                  reads=[xt_t], st=xt_t)
            yield

        def pull(gen, k):
            if gen is None:
                return None
            for _ in range(k):
                try:
                    next(gen)
                except StopIteration:
                    return None
            return gen

        cur = load_x(0)
        stage_A2(0, *cur)
        dgen = None
        for s in range(NS2):
            xt, xt_t = cur
            xnT, xnT_t = xnTs[s % 2], xnT_ts[s % 2]
            hT, hT_t = hTs[s % 2], hT_ts[s % 2]
            if s + 1 < NS2:
                nxt = load_x(s + 1)
            for f in range(NFF):
                uu = []
                for ff in (f, NFF + f):
                    pb, pb_t = pb_pool.get()
                    for c in range(8):
                        P.op("pe", lambda e, c=c, ff=ff, pb=pb: e.matmul(pb[:, 0:T2], lhsT=wup[:, c, ff * 128:(ff + 1) * 128],
                                                                       rhs=xnT[:, c, :], start=(c == 0), stop=(c == 7)),
                             reads=[xnT_t, wup_t[ff // 4]], writes=[pb_t], inc=(c == 7))
                    upb, upb_t = upb_pool.get()
                    P.op("pool", lambda e, upb=upb, ff=ff: e.tensor_copy(out=upb[:, 0:2], in_=hist[:, ff, :]),
                         reads=[hist_t[ff]], writes=[upb_t])
                    P.op("act", lambda e, upb=upb, pb=pb: e.activation(out=upb[:, 2:2 + T2], in_=pb[:, 0:T2], func=AF.Copy),
                         reads=[pb_t], writes=[upb_t])
                    P.op("pool", lambda e, upb=upb, ff=ff: e.tensor_copy(out=hist[:, ff, :], in_=upb[:, T2:T2 + 2]),
                         reads=[upb_t], writes=[hist_t[ff]])
                    u, u_t = u_pool.get()
                    P.op("act", lambda e, u=u, pb=pb, ff=ff: e.activation(
                        out=u[:], in_=pb[:, 0:T2], func=AF.Identity, scale=cv[:, fcw + 2 * 44 + ff:fcw + 2 * 44 + ff + 1],
                        bias=cv[:, fcb + ff:fcb + ff + 1]),
                        reads=[pb_t, cv_t], writes=[u_t])
                    for j in (1, 0):
                        P.op("dve", lambda e, u=u, upb=upb, ff=ff, j=j: e.scalar_tensor_tensor(
                            out=u[:], in0=upb[:, j:j + T2], scalar=cv[:, fcw + j * 44 + ff:fcw + j * 44 + ff + 1],
                            in1=u[:], op0=ALU.mult, op1=ALU.add),
                            reads=[upb_t, cv_t], writes=[u_t])
                    uu.append((u, u_t))
                sg, sg_t = sg_pool.get()
                P.op("act", lambda e, sg=sg, u=uu[0][0]: e.activation(out=sg[:], in_=u[:], func=AF.Silu),
                     reads=[uu[0][1]], writes=[sg_t])
                P.op("dve", lambda e, sg=sg, u=uu[1][0], f=f: e.tensor_tensor(out=hT[:, f, :], in0=sg[:], in1=u[:], op=ALU.mult),
                     reads=[sg_t, uu[1][1]], writes=[hT_t])
                dgen = pull(dgen, 4 if f < NFF - 1 else 1000)
                if f == 8 and s + 1 < NS2:
                    stage_A2(s + 1, *nxt)
            dgen = down_ops(s, xt, xt_t)
            if s + 1 < NS2:
                cur = nxt
        pull(dgen, 1000)
        P.wait_all("sp", [t for _, t in xt_pool.items])


def pack_cvec(inp):
    cvv = np.zeros((128, NV), np.float32)

    def put(name, vec, width):
        cvv[:, CV[name]:CV[name] + width] = np.asarray(vec, np.float32).reshape(width, 128).T

    put("g1", inp["norm1_g"][0], 8)
    put("g2", inp["norm2_g"][0], 8)
    put("convw", inp["conv_w"][0].reshape(-1), 32)
    put("convb", inp["conv_b"][0], 8)
    put("ba", inp["rg_ba"][0], 8)
    put("bx", inp["rg_bx"][0], 8)
    put("lam", inp["rg_lambda"][0], 8)
    put("fcw", inp["ffn_conv_w"][0].reshape(-1), 132)
    put("fcb", inp["ffn_conv_b"][0], 44)
    cvv[0:64, CV["qg"]] = np.asarray(inp["q_norm_g"][0], np.float32)
    cvv[0:64, CV["kg"]] = np.asarray(inp["k_norm_g"][0], np.float32)
    cvv[0:64, CV["kig"]] = np.asarray(inp["kidx_norm_g"][0], np.float32)
    return cvv


def make_in_maps(inp):
    cvv = pack_cvec(inp)
    cst = np.zeros((128, 256), np.float32)
    cst[:, 0:128] = np.eye(128, dtype=np.float32)
    cst[:, 128:256] = np.where(np.arange(128)[None, :] <= np.arange(128)[:, None], 0.0, -1e30)
    shared = {
        "cvec": cvv,
        "consts": cst,
        "w_in": np.ascontiguousarray(inp["w_in"][0], dtype=np.float32),
        "rg_wa": np.ascontiguousarray(inp["rg_wa"][0], dtype=np.float32),
        "rg_wx": np.ascontiguousarray(inp["rg_wx"][0], dtype=np.float32),
        "w_o_attn": np.ascontiguousarray(inp["w_o_attn"][0], dtype=np.float32),
        "w_o_rnn": np.ascontiguousarray(inp["w_o_rnn"][0], dtype=np.float32),
        "w_out": np.ascontiguousarray(inp["w_out"][0], dtype=np.float32),
        "w_up": np.ascontiguousarray(inp["w_up"][0], dtype=np.float32),
        "w_down": np.ascontiguousarray(inp["w_down"][0], dtype=np.float32),
    }
    x = np.asarray(inp["x"], np.float32)
    maps = []
    for b in range(NCORES):
        m = dict(shared)
        m["x"] = np.ascontiguousarray(x[b])
        maps.append(m)
    return maps


def kernel(**inputs):
    nc = build_nc()
    in_maps = make_in_maps(inputs)
    res = run_bass_kernel_spmd(nc, in_maps, core_ids=list(range(NCORES)))
    return np.stack([np.asarray(r["out"], np.float32) for r in res.results], axis=0)
```
